# Optimizing a Trainium2 kernel written in Bass

```python
import math
import jax, jax.numpy as jnp
from jax import lax
import numpy as np

D_MODEL = 1024
BATCH = 8
SEQ = 4096
DEPTH = 2

N_META = 16
BLOCK = 128
LEAD = BLOCK
N_PAD = LEAD - N_META
ATTN_HEADS = 8
ATTN_KV_HEADS = 2
HEAD_DIM = 64
WINDOW = BLOCK
ATTN_GROUP = ATTN_HEADS // ATTN_KV_HEADS
ATTN_WIDTH = ATTN_HEADS * HEAD_DIM
KV_WIDTH = ATTN_KV_HEADS * HEAD_DIM
CONV_WIDTH = 512
CONV_KERNEL = 31
MLSTM_HEADS = 4
MLSTM_WIDTH = 512
MLSTM_HEAD_DIM = MLSTM_WIDTH // MLSTM_HEADS
MLSTM_CHUNK = 64
QK_CONV_KERNEL = 4
D_FF = 2816
N_BRANCH = 3
SPLIT_SIZES = (ATTN_WIDTH, KV_WIDTH, KV_WIDTH,
               CONV_WIDTH, CONV_WIDTH,
               MLSTM_WIDTH, MLSTM_WIDTH, MLSTM_WIDTH, MLSTM_WIDTH,
               MLSTM_HEADS, MLSTM_HEADS,
               N_BRANCH * D_MODEL)
N_IN = sum(SPLIT_SIZES)
RMS_EPS = 1e-6
LN_EPS = 1e-5
NEG = -1e30

kernel_name = "hybrid_swa_conformer_mlstm_macaron"


def rms_norm(x, g):
    x32 = x.astype(jnp.float32)
    y = x32 * lax.rsqrt(jnp.mean(x32 * x32, axis=-1, keepdims=True) + RMS_EPS)
    return (y * g.astype(jnp.float32)).astype(x.dtype)


def layer_norm(x, g, b):
    x32 = x.astype(jnp.float32)
    mu = jnp.mean(x32, axis=-1, keepdims=True)
    xc = x32 - mu
    y = xc * lax.rsqrt(jnp.mean(xc * xc, axis=-1, keepdims=True) + LN_EPS)
    return (y * g.astype(jnp.float32) + b.astype(jnp.float32)).astype(x.dtype)


def causal_depthwise_conv(x, w, b, valid):
    x = jnp.where(valid[None, :, None], x, jnp.zeros((), x.dtype))
    ksz = w.shape[0]
    y = lax.conv_general_dilated(
        x, w[:, None, :].astype(x.dtype), window_strides=(1,),
        padding=[(ksz - 1, 0)], dimension_numbers=("NWC", "WIO", "NWC"),
        feature_group_count=x.shape[-1])
    return y + b.astype(x.dtype)


def swiglu_ffn(x, g, w1, w3, w2):
    h = rms_norm(x, g)
    return (jax.nn.silu(h @ w1) * (h @ w3)) @ w2


def sliding_window_attention(q, k, v, q_gain, k_gain, sinks, valid):
    bsz, t_len = q.shape[:2]
    nb = t_len // BLOCK
    q = rms_norm(q, q_gain).astype(jnp.float32) * (HEAD_DIM ** -0.5)
    k = rms_norm(k, k_gain).astype(jnp.float32)
    v32 = v.astype(jnp.float32)
    qb = q.reshape(bsz, nb, BLOCK, ATTN_KV_HEADS, ATTN_GROUP, HEAD_DIM)

    def with_prev(t):
        tb = t.reshape(bsz, nb, BLOCK, ATTN_KV_HEADS, HEAD_DIM)
        prev = jnp.pad(tb[:, :-1], [(0, 0), (1, 0), (0, 0), (0, 0), (0, 0)])
        return jnp.concatenate([prev, tb], axis=2)

    kb, vb = with_prev(k), with_prev(v32)
    vblk = valid.reshape(nb, BLOCK)
    kvalid = jnp.concatenate([jnp.pad(vblk[:-1], [(1, 0), (0, 0)]), vblk], axis=1)
    qpos = jnp.arange(BLOCK)[:, None] + BLOCK
    kpos = jnp.arange(2 * BLOCK)[None, :]
    band = (kpos <= qpos) & (qpos - kpos < WINDOW)
    mask = band[None] & kvalid[:, None, :]
    s = jnp.einsum("bnqhgd,bnkhd->bnhgqk", qb, kb)
    s = jnp.where(mask[None, :, None, None], s, NEG)
    sink = jnp.broadcast_to(
        sinks.astype(jnp.float32).reshape(ATTN_KV_HEADS, ATTN_GROUP)[None, None, :, :, None, None],
        s.shape[:-1] + (1,))
    p = jax.nn.softmax(jnp.concatenate([s, sink], axis=-1), axis=-1)[..., :-1]
    o = jnp.einsum("bnhgqk,bnkhd->bnqhgd", p, vb)
    return o.reshape(bsz, t_len, ATTN_WIDTH).astype(q_gain.dtype)


def mlstm_chunkwise(q, k, v, log_i, log_f):
    bsz, t_len, nh, d = q.shape
    L = MLSTM_CHUNK
    nc = t_len // L

    def chunk(t):
        return t.reshape(bsz, nc, L, nh, d).transpose(0, 3, 1, 2, 4)

    qc = chunk(q) * (d ** -0.5)
    kc, vc = chunk(k), chunk(v)
    li = log_i.reshape(bsz, nc, L, nh).transpose(0, 3, 1, 2)
    lf = log_f.reshape(bsz, nc, L, nh).transpose(0, 3, 1, 2)
    bcum = jnp.cumsum(lf, axis=-1)
    b_end = bcum[..., -1]
    causal = jnp.tril(jnp.ones((L, L), dtype=bool))
    log_d = jnp.where(causal, bcum[..., :, None] - bcum[..., None, :] + li[..., None, :], NEG)

    log_e = b_end[..., None] - bcum + li
    m_loc = jnp.max(log_e, axis=-1)
    w_e = jnp.exp(log_e - m_loc[..., None])
    c_loc = jnp.einsum("bhcs,bhcsd,bhcse->bhcde", w_e, vc, kc)
    n_loc = jnp.einsum("bhcs,bhcse->bhce", w_e, kc)

    def step(carry, xs):
        c_st, n_st, m_st = carry
        c_l, n_l, m_l, b_l = xs
        m_new = jnp.maximum(b_l + m_st, m_l)
        a = jnp.exp(b_l + m_st - m_new)
        cc = jnp.exp(m_l - m_new)
        c_new = a[..., None, None] * c_st + cc[..., None, None] * c_l
        n_new = a[..., None] * n_st + cc[..., None] * n_l
        return (c_new, n_new, m_new), (c_st, n_st, m_st)

    init = (jnp.zeros((bsz, nh, d, d), jnp.float32), jnp.zeros((bsz, nh, d), jnp.float32),
            jnp.full((bsz, nh), NEG, jnp.float32))
    xs = (jnp.moveaxis(c_loc, 2, 0), jnp.moveaxis(n_loc, 2, 0),
          jnp.moveaxis(m_loc, 2, 0), jnp.moveaxis(b_end, 2, 0))
    _, (c_prev, n_prev, m_prev) = lax.scan(step, init, xs)
    c_prev = jnp.moveaxis(c_prev, 0, 2)
    n_prev = jnp.moveaxis(n_prev, 0, 2)
    m_prev = jnp.moveaxis(m_prev, 0, 2)

    m_inter = bcum + m_prev[..., None]
    m_t = jnp.maximum(m_inter, jnp.max(log_d, axis=-1))
    w_inter = jnp.exp(m_inter - m_t)
    s = jnp.einsum("bhctd,bhcsd->bhcts", qc, kc) * jnp.exp(log_d - m_t[..., None])
    num = jnp.einsum("bhcts,bhcsd->bhctd", s, vc) + \
        w_inter[..., None] * jnp.einsum("bhcde,bhcte->bhctd", c_prev, qc)
    den = jnp.sum(s, axis=-1) + w_inter * jnp.einsum("bhce,bhcte->bhct", n_prev, qc)
    h = num / jnp.maximum(jnp.abs(den), jnp.exp(-m_t))[..., None]
    return h.transpose(0, 2, 3, 1, 4).reshape(bsz, t_len, nh * d)


def hybrid_mixer(x, valid, mix_norm, w_in, gate_bias, attn_q_norm, attn_k_norm, attn_sinks,
                 w_o_attn, conv_dw_w, conv_dw_b, conv_ln_g, conv_ln_b, w_o_conv,
                 mlstm_qk_conv_w, mlstm_qk_conv_b, mlstm_igate_bias, mlstm_fgate_bias,
                 w_o_mlstm, w_out):
    bsz, t_len, _ = x.shape
    h = rms_norm(x, mix_norm)
    z = h @ w_in
    parts = []
    off = 0
    for n in SPLIT_SIZES:
        parts.append(z[..., off:off + n])
        off += n
    aq, ak, av, ca, cg, mq, mk, mv, mo, mi, mf, zg = parts

    ya = sliding_window_attention(
        aq.reshape(bsz, t_len, ATTN_HEADS, HEAD_DIM),
        ak.reshape(bsz, t_len, ATTN_KV_HEADS, HEAD_DIM),
        av.reshape(bsz, t_len, ATTN_KV_HEADS, HEAD_DIM),
        attn_q_norm, attn_k_norm, attn_sinks, valid).astype(x.dtype) @ w_o_attn

    u = ca * jax.nn.sigmoid(cg)
    u = causal_depthwise_conv(u, conv_dw_w, conv_dw_b, valid)
    u = jax.nn.silu(layer_norm(u, conv_ln_g, conv_ln_b))
    yc = u @ w_o_conv

    qk = jax.nn.silu(causal_depthwise_conv(jnp.concatenate([mq, mk], axis=-1),
                                           mlstm_qk_conv_w, mlstm_qk_conv_b, valid))
    qm, km = qk[..., :MLSTM_WIDTH], qk[..., MLSTM_WIDTH:]
    log_i = mi.astype(jnp.float32) + mlstm_igate_bias.astype(jnp.float32)
    log_i = jnp.where(valid[None, :, None], log_i, NEG)
    log_f = jax.nn.log_sigmoid(mf.astype(jnp.float32) + mlstm_fgate_bias.astype(jnp.float32))
    shp = (bsz, t_len, MLSTM_HEADS, MLSTM_HEAD_DIM)
    h_tilde = mlstm_chunkwise(qm.reshape(shp).astype(jnp.float32),
                              km.reshape(shp).astype(jnp.float32),
                              mv.reshape(shp).astype(jnp.float32), log_i, log_f)
    ym = (jax.nn.sigmoid(mo) * h_tilde.astype(x.dtype)) @ w_o_mlstm

    g = jax.nn.sigmoid(zg.reshape(bsz, t_len, N_BRANCH, D_MODEL) + gate_bias)
    y = g[:, :, 0] * ya + g[:, :, 1] * yc + g[:, :, 2] * ym
    return y @ w_out


def setup_inputs(seed: int = 0) -> dict:
    key = jax.random.key(seed)
    ks = jax.random.split(key, 32)
    f32 = jnp.float32

    def nrm(k, shape, scale):
        return jax.random.normal(k, shape, f32) * scale

    def gain(k, shape):
        return 1.0 + 0.05 * jax.random.normal(k, shape, f32)

    L = DEPTH
    return {
        "x": jax.random.normal(ks[0], (BATCH, SEQ, D_MODEL), f32),
        "meta_tokens": nrm(ks[1], (N_META, D_MODEL), 1.0),
        "ffn1_norm": gain(ks[2], (L, D_MODEL)),
        "ffn1_w1": nrm(ks[3], (L, D_MODEL, D_FF), D_MODEL ** -0.5),
        "ffn1_w3": nrm(ks[4], (L, D_MODEL, D_FF), D_MODEL ** -0.5),
        "ffn1_w2": nrm(ks[5], (L, D_FF, D_MODEL), D_FF ** -0.5),
        "mix_norm": gain(ks[6], (L, D_MODEL)),
        "w_in": nrm(ks[7], (L, D_MODEL, N_IN), D_MODEL ** -0.5),
        "gate_bias": nrm(ks[8], (L, N_BRANCH, D_MODEL), 0.1),
        "attn_q_norm": gain(ks[9], (L, HEAD_DIM)),
        "attn_k_norm": gain(ks[10], (L, HEAD_DIM)),
        "attn_sinks": nrm(ks[11], (L, ATTN_HEADS), 0.5),
        "w_o_attn": nrm(ks[12], (L, ATTN_WIDTH, D_MODEL), ATTN_WIDTH ** -0.5),
        "conv_dw_w": nrm(ks[13], (L, CONV_KERNEL, CONV_WIDTH), CONV_KERNEL ** -0.5),
        "conv_dw_b": nrm(ks[14], (L, CONV_WIDTH), 0.02),
        "conv_ln_g": gain(ks[15], (L, CONV_WIDTH)),
        "conv_ln_b": nrm(ks[16], (L, CONV_WIDTH), 0.02),
        "w_o_conv": nrm(ks[17], (L, CONV_WIDTH, D_MODEL), CONV_WIDTH ** -0.5),
        "mlstm_qk_conv_w": nrm(ks[18], (L, QK_CONV_KERNEL, 2 * MLSTM_WIDTH), QK_CONV_KERNEL ** -0.5),
        "mlstm_qk_conv_b": nrm(ks[19], (L, 2 * MLSTM_WIDTH), 0.02),
        "mlstm_igate_bias": nrm(ks[20], (L, MLSTM_HEADS), 0.1),
        "mlstm_fgate_bias": 3.0 + nrm(ks[21], (L, MLSTM_HEADS), 0.5),
        "w_o_mlstm": nrm(ks[22], (L, MLSTM_WIDTH, D_MODEL), MLSTM_WIDTH ** -0.5),
        "w_out": nrm(ks[23], (L, D_MODEL, D_MODEL), D_MODEL ** -0.5),
        "ffn2_norm": gain(ks[24], (L, D_MODEL)),
        "ffn2_w1": nrm(ks[25], (L, D_MODEL, D_FF), D_MODEL ** -0.5),
        "ffn2_w3": nrm(ks[26], (L, D_MODEL, D_FF), D_MODEL ** -0.5),
        "ffn2_w2": nrm(ks[27], (L, D_FF, D_MODEL), D_FF ** -0.5),
    }


def reference(x, meta_tokens, ffn1_norm, ffn1_w1, ffn1_w3, ffn1_w2, mix_norm, w_in, gate_bias,
              attn_q_norm, attn_k_norm, attn_sinks, w_o_attn, conv_dw_w, conv_dw_b, conv_ln_g,
              conv_ln_b, w_o_conv, mlstm_qk_conv_w, mlstm_qk_conv_b, mlstm_igate_bias,
              mlstm_fgate_bias, w_o_mlstm, w_out, ffn2_norm, ffn2_w1, ffn2_w3, ffn2_w2):
    bsz = x.shape[0]
    lead = jnp.concatenate([
        jnp.zeros((bsz, N_PAD, D_MODEL), x.dtype),
        jnp.broadcast_to(meta_tokens.astype(x.dtype)[None], (bsz, N_META, D_MODEL))], axis=1)
    h = jnp.concatenate([lead, x], axis=1)
    valid = jnp.arange(h.shape[1]) >= N_PAD
    for l in range(DEPTH):
        h = h + 0.5 * swiglu_ffn(h, ffn1_norm[l], ffn1_w1[l], ffn1_w3[l], ffn1_w2[l])
        h = h + hybrid_mixer(h, valid, mix_norm[l], w_in[l], gate_bias[l], attn_q_norm[l],
                             attn_k_norm[l], attn_sinks[l], w_o_attn[l], conv_dw_w[l],
                             conv_dw_b[l], conv_ln_g[l], conv_ln_b[l], w_o_conv[l],
                             mlstm_qk_conv_w[l], mlstm_qk_conv_b[l], mlstm_igate_bias[l],
                             mlstm_fgate_bias[l], w_o_mlstm[l], w_out[l])
        h = h + 0.5 * swiglu_ffn(h, ffn2_norm[l], ffn2_w1[l], ffn2_w3[l], ffn2_w2[l])
    return h[:, LEAD:]
```

```python
import math
from collections import deque
from contextlib import ExitStack

import numpy as np
import concourse.bass as bass
import concourse.mybir as mybir
from concourse.bass_utils import run_bass_kernel_spmd

F32 = mybir.dt.float32
BF16 = mybir.dt.bfloat16
AF = mybir.ActivationFunctionType
ALU = mybir.AluOpType

D = 1024
KC = 8
DFF = 2816
NIN = 6920
NMETA = 16
DEPTH = 2
ENGS = ("pe", "act", "dve", "pool", "sp")
STOP = None
NSW = 40

PV_F1G, PV_MXG, PV_F2G, PV_GB, PV_CW, PV_CB, PV_LG, PV_LB, PV_QW, PV_QB, PV_GQ, PV_GK, PV_SK, PV_GT, NV = (
    0, 8, 16, 24, 48, 172, 176, 180, 184, 216, 224, 225, 226, 230, 238)


class Res:
    __slots__ = ("lw", "rd")

    def __init__(self):
        self.lw = []
        self.rd = {}


def mkres(n):
    return [Res() for _ in range(n)]


class Prog:
    def __init__(self):
        self.st = {e: [] for e in ENGS}
        self.seen = {e: {} for e in ENGS}
        self.dmacnt = {}
        self.nsw = 0

    def add(self, eng, fn, reads=(), writes=(), dmakey=None, lw_only=()):
        st = self.st[eng]
        idx = len(st)
        need = {}
        isdma = dmakey is not None
        if dmakey is None:
            tok = ("e", eng, idx)
        else:
            if eng == "pool":
                dmakey = ("sw", self.nsw % NSW)
                self.nsw += 1
                prev = self.dmacnt.get(dmakey, 0)
                if prev:
                    need[("d", dmakey)] = prev
            c = self.dmacnt.get(dmakey, 0) + 1
            self.dmacnt[dmakey] = c
            tok = ("d", dmakey, c)

        def want(t, raw):
            if t is None:
                return
            kind, key, val = t
            if kind == "e" and key == eng and not isdma:
                if (not raw) or eng == "pe":
                    return
            k = (kind, key)
            if need.get(k, -1) < val:
                need[k] = val

        for r in reads:
            for t_ in r.lw:
                want(t_, True)
        for w in writes:
            for t_ in w.lw:
                want(t_, False)
            for k, v in w.rd.items():
                want((k[0], k[1], v), False)
        waits = []
        sn = self.seen[eng]
        for k, v in need.items():
            if sn.get(k, -1) < v:
                sn[k] = v
                waits.append((k[0], k[1], v))
        st.append((fn, waits, dmakey))
        k = (tok[0], tok[1])
        for r in reads:
            if r.rd.get(k, -1) < tok[2]:
                r.rd[k] = tok[2]
        for w in writes:
            w.lw = [tok]
            w.rd = {}
        for w in lw_only:
            w.lw.append(tok)
        return tok

    def emit(self, nc, es, final_dma_keys):
        ranks = {e: {} for e in ENGS}
        used = {e: set() for e in ENGS}
        for e in ENGS:
            for (_, waits, _) in self.st[e]:
                for kind, key, val in waits:
                    if kind == "e":
                        used[key].add(val)
        for e in ENGS:
            for i, idx in enumerate(sorted(used[e])):
                ranks[e][idx] = i + 1
        esem = {e: es.enter_context(nc.semaphore("sem_" + e)) for e in ENGS}
        dsem = {k: es.enter_context(nc.semaphore("dsem%d" % n)) for n, k in enumerate(self.dmacnt)}
        block = es.enter_context(nc.Block())
        st = self.st
        dmacnt = self.dmacnt

        def run(e, eng):
            rk = ranks[eng]
            for i, (fn, waits, dmakey) in enumerate(st[eng]):
                for kind, key, val in waits:
                    if kind == "e":
                        e.wait_ge(esem[key], ranks[key][val])
                    else:
                        e.wait_ge(dsem[key], 16 * val)
                ins = fn(e)
                if dmakey is not None:
                    ins.then_inc(dsem[dmakey], 16)
                elif i in rk:
                    ins.then_inc(esem[eng], 1)
            if eng == "sp":
                for k in final_dma_keys:
                    if k in dmacnt:
                        e.wait_ge(dsem[k], 16 * dmacnt[k])

        @block.tensor
        def _(e):
            run(e, "pe")

        @block.scalar
        def _(e):
            run(e, "act")

        @block.vector
        def _(e):
            run(e, "dve")

        @block.gpsimd
        def _(e):
            run(e, "pool")

        @block.sync
        def _(e):
            run(e, "sp")


class Pool:
    def __init__(self, items):
        self.free = deque(items)

    def get(self):
        assert self.free, "scratch pool exhausted"
        return self.free.popleft()

    def put(self, it):
        self.free.append(it)


class Builder:
    def __init__(self, seq, depth=DEPTH, nt_max=512):
        self.seq = seq
        self.depth = depth
        tot = NMETA + seq
        self.tiles = []
        pos = 0
        while pos < tot:
            nb = min(nt_max // 128, (tot - pos + 127) // 128)
            self.tiles.append((pos, nb))
            pos += nb * 128
        self.P = Prog()

    def mm(self, out, lhsT, rhs, start, stop, reads, writes):
        self.P.add("pe", lambda e: e.matmul(out, lhsT=lhsT, rhs=rhs, start=start, stop=stop), reads, writes)

    def tr(self, out, in_, ident, reads, writes):
        self.P.add("pe", lambda e: e.transpose(out, in_, ident), reads, writes)

    def act(self, out, in_, func, reads, writes, bias=None, scale=None):
        kw = {}
        if bias is not None:
            kw["bias"] = bias
        if scale is not None:
            kw["scale"] = scale
        self.P.add("act", lambda e: e.activation(out=out, in_=in_, func=func, **kw), reads, writes)

    def tt(self, out, in0, in1, op, reads, writes, eng="dve"):
        self.P.add(eng, lambda e: e.tensor_tensor(out=out, in0=in0, in1=in1, op=op), reads, writes)

    def ts(self, out, in0, s1, s2, op0, op1, reads, writes, eng="dve"):
        if s2 is None:
            self.P.add(eng, lambda e: e.tensor_scalar(out=out, in0=in0, scalar1=s1, scalar2=None, op0=op0), reads, writes)
        else:
            self.P.add(eng, lambda e: e.tensor_scalar(out=out, in0=in0, scalar1=s1, scalar2=s2, op0=op0, op1=op1), reads, writes)

    def stt(self, out, in0, scalar, in1, op0, op1, reads, writes, eng="dve"):
        self.P.add(eng, lambda e: e.scalar_tensor_tensor(out=out, in0=in0, scalar=scalar, in1=in1, op0=op0, op1=op1), reads, writes)

    def cp(self, out, in_, reads, writes, eng="dve"):
        self.P.add(eng, lambda e: e.tensor_copy(out=out, in_=in_), reads, writes)

    def recip(self, out, in_, reads, writes):
        self.P.add("dve", lambda e: e.reciprocal(out=out, in_=in_), reads, writes)

    def memset(self, ap, val, writes, eng="dve"):
        self.P.add(eng, lambda e: e.memset(ap, val), (), writes)

    def dma(self, eng, out, in_, key, reads, writes, lw_only=()):
        self.P.add(eng, lambda e: e.dma_start(out=out, in_=in_), reads, writes, dmakey=key, lw_only=lw_only)

    def slot_dmas(self, i, res, pairs):
        for n, (dst, src) in enumerate(pairs):
            if n == 0:
                self.dma("pool", dst, src, ("w", i), [], [res])
            else:
                self.dma("pool", dst, src, ("w", i), [], [], lw_only=[res])

    def sigmoid(self, out, in_, reads, wres, negbias=None):
        self.act(out, in_, AF.Exp, reads, [wres], scale=-1.0, bias=negbias)
        self.act(out, out, AF.Ln, [wres], [wres], bias=1.0)
        self.act(out, out, AF.Exp, [wres], [wres], scale=-1.0)

    def rsqrt(self, out, in_, scale, eps, reads, wres, lnmul=0.0):
        self.act(out, in_, AF.Ln, reads, [wres], scale=scale, bias=eps)
        if lnmul != 0.0:
            self.act(out, out, AF.Exp, [wres], [wres], scale=-0.5, bias=lnmul)
        else:
            self.act(out, out, AF.Exp, [wres], [wres], scale=-0.5)

    def build(self):
        nc = bass.Bass("TRN2", target_bir_lowering=False)
        self.nc = nc
        seq = self.seq
        L = DEPTH
        dr = {}

        def din(name, shape):
            dr[name] = nc.dram_tensor(name, list(shape), F32, kind="ExternalInput").ap()

        din("xin", (seq, D))
        din("meta", (NMETA, D))
        for f in ("ffn1", "ffn2"):
            din(f + "_w1", (L, D, DFF))
            din(f + "_w3", (L, D, DFF))
            din(f + "_w2", (L, DFF, D))
        din("w_in", (L, D, NIN))
        din("w_o_attn", (L, 512, D))
        din("w_o_conv", (L, 512, D))
        din("w_o_mlstm", (L, 512, D))
        din("w_out", (L, D, D))
        din("pv", (L, 128, NV))
        din("c32", (128, 1408))
        din("cbf", (128, 1408))
        dr["out"] = nc.dram_tensor("out", [seq, D], F32, kind="ExternalOutput").ap()
        self.dr = dr

        with ExitStack() as es:
            self.es = es

            def sb(name, shape, dt):
                return es.enter_context(nc.sbuf_tensor("sb_" + name, list(shape), dt))

            T = self
            T.c32 = sb("c32", [128, 1408], F32)
            T.cbf = sb("cbf", [128, 1408], BF16)
            T.pvt = sb("pvt", [128, L, NV], F32)
            T.ngb = sb("ngb", [128, L, 24], F32)
            T.sinkx = sb("sinkx", [128, L, 512], F32)
            T.sinke = sb("sinke", [128, L, 4], F32)
            T.xT = sb("xT", [128, KC, 512], F32)
            T.hT = sb("hT", [128, KC, 512], BF16)
            T.xs = sb("xs", [128, 2, D], F32)
            NSLOT = 5
            T.ring = sb("ring", [128, NSLOT, 4096], BF16)
            T.m = sb("m", [128, 11, 512], BF16)
            T.scf = sb("scf", [128, 6, 512], F32)
            T.scb = sb("scb", [128, 6, 520], BF16)
            T.qn = sb("qn", [128, 4, 512], BF16)
            T.kT = sb("kT", [128, L, 128 + 512], BF16)
            T.vtk = sb("vtk", [128, L, 5, 128], BF16)
            T.oa = sb("oa", [128, 4, 512], BF16)
            T.u = sb("u", [128, L, 4, 30 + 512], F32)
            T.acc = sb("acc", [128, 4, 512], F32)
            T.cb = sb("cb", [128, 4, 512], BF16)
            T.raw = sb("raw", [128, 2, 3 + 512], F32)
            T.qkh = sb("qkh", [128, L, 8, 3], F32)
            T.cacc = sb("cacc", [128, 2, 512], F32)
            T.mqk = sb("mqk", [128, 8, 512], BF16)
            T.ktok = sb("ktok", [128, 4, 4, 128], BF16)
            T.vaug = sb("vaug", [128, 4, 4, 130], BF16)
            T.so = sb("so", [128, 4, 512], BF16)
            T.hm = sb("hm", [128, 4, 512], BF16)
            T.Cst = sb("Cst", [128, L, 4, 129], F32)
            T.Cbf = sb("Cbf", [128, L, 4, 128], BF16)
            T.nbc = sb("nbc", [128, L, 4, 128], BF16)
            T.sm = sb("sm", [128, 8, 8], F32)
            T.mg = sb("mg", [128, 8, 512], BF16)
            ps = [es.enter_context(nc.psum_tensor("ps%d" % i, [128, 512], F32)) for i in range(8)]

            R = {}
            for nm, n in (("c", 1), ("pv", 1), ("xT", KC), ("hT", KC), ("xs", 2), ("m", 11), ("qn", 4), ("kT", L),
                          ("vtk", L), ("oa", 4), ("u", L * 4), ("acc", 4), ("cb", 4), ("raw", 2), ("qkh", L), ("cacc", 2),
                          ("mqk", 8), ("ktok", 4), ("vaug", 4), ("so", 4), ("hm", 4), ("Cst", L), ("Cbf", L), ("nbc", L),
                          ("sm", 2), ("mg", 8)):
                R[nm] = mkres(n)
            T.R = R
            T.pspool = Pool([(ps[i], Res()) for i in range(8)])
            T.fpool = Pool([(i, Res()) for i in range(6)])
            T.bpool = Pool([(i, Res()) for i in range(6)])
            T.slots = [(i, Res()) for i in range(NSLOT)]
            T.slot_next = 0

            self.setup()
            for ti, (pos, nb) in enumerate(self.tiles):
                self.load_tile(ti, pos, nb)
                self.stopped = (STOP == "load")
                for l in range(self.depth):
                    if not self.stopped:
                        self.ffn(l, 1, nb)
                        self.stopped = (STOP == "ffn1")
                    if not self.stopped:
                        self.mixer(l, nb, first=(ti == 0))
                    if not self.stopped:
                        self.ffn(l, 2, nb)
                self.store_tile(ti, pos, nb)
            self.P.emit(nc, es, [("out", 0), ("out", 1)])
        return nc

    def piece(self, dmas):
        i, res = self.slots[self.slot_next % len(self.slots)]
        self.slot_next += 1
        base = self.ring[:, i, :]
        self.slot_dmas(i, res, [(dst_fn(base), src) for dst_fn, src in dmas])
        return base, res

    def setup(self):
        T = self
        dr = T.dr
        R = T.R
        c = R["c"][0]
        self.dma("sp", T.c32[:], dr["c32"], "c32", [], [c])
        self.dma("pool", T.cbf[:], dr["cbf"], "cbf", [], [], lw_only=[c])
        self.dma("sp", T.pvt[:], dr["pv"].rearrange("l p n -> p l n"), "pv", [], [R["pv"][0]])
        T.ident_f = T.c32[:, 0:128]
        T.tri_f = T.c32[:, 128:256]
        T.maskb4 = T.c32[:, 256:768]
        T.ones_f = T.c32[:, 768:896]
        T.ident4_f = T.c32[:, 896:1408]
        T.ones_b = T.cbf[:, 0:128]
        T.blk_b = T.cbf[:, 128:256]
        T.ident_b = T.cbf[:, 256:384]
        T.mown4 = T.cbf[:, 384:896]
        T.mprev4 = T.cbf[:, 896:1408]
        pvr = R["pv"][0]
        for l in range(DEPTH):
            self.ts(T.ngb[:, l, :], T.pvt[:, l, PV_GB:PV_GB + 24], -1.0, None, ALU.mult, None, [pvr], [pvr])
            self.act(T.sinke[:, l, :], T.pvt[:, l, PV_SK:PV_SK + 4], AF.Exp, [pvr], [pvr])
            for j in range(4):
                self.ts(T.sinkx[:, l, j * 128:(j + 1) * 128], T.ones_f, T.sinke[:, l, j:j + 1], None, ALU.mult, None,
                        [pvr, c], [pvr])
            self.memset(T.kT[:, l, 0:128], 0.0, [R["kT"][l]])
            self.memset(T.vtk[:, l, 0, :], 0.0, [R["vtk"][l]])
            for cc in range(4):
                self.memset(T.u[:, l, cc, 0:30], 0.0, [R["u"][l * 4 + cc]])
            self.memset(T.qkh[:, l, :, :], 0.0, [R["qkh"][l]])
            self.memset(T.Cst[:, l, :, :], 0.0, [R["Cst"][l]])
            self.memset(T.Cbf[:, l, :, :], 0.0, [R["Cbf"][l]])
            self.memset(T.nbc[:, l, :, :], 0.0, [R["nbc"][l]])
        for b in range(4):
            self.memset(T.vaug[:, b, :, 128:129], 1.0, [R["vaug"][b]])

    def load_tile(self, ti, pos, nb):
        T = self
        dr = T.dr
        R = T.R
        nt = nb * 128
        tot = NMETA + T.seq
        for b in range(nb):
            p0 = pos + b * 128
            sl = b % 2
            xr = R["xs"][sl]
            nvalid = max(0, min(128, tot - p0))
            if nvalid < 128:
                self.memset(T.xs[:, sl, :], 0.0, [xr])
            if p0 == 0:
                self.dma("sp", T.xs[0:NMETA, sl, :], dr["meta"], ("xs", sl), [], [xr])
                self.dma("sp", T.xs[NMETA:128, sl, :], dr["xin"][0:128 - NMETA, :], ("xs", sl), [], [], lw_only=[xr])
            elif nvalid > 0:
                r0 = p0 - NMETA
                self.dma("sp", T.xs[0:nvalid, sl, :], dr["xin"][r0:r0 + nvalid, :], ("xs", sl), [], [xr])
            for h in range(2):
                pst, psr = T.pspool.get()
                for q in range(4):
                    kc = h * 4 + q
                    self.tr(pst[:, q * 128:(q + 1) * 128], T.xs[:, sl, kc * 128:(kc + 1) * 128], T.ident_f,
                            [xr, R["c"][0]], [psr])
                dst = T.xT[:, h * 4:(h + 1) * 4, b * 128:(b + 1) * 128]
                src = pst[:, :].rearrange("p (q t) -> p q t", q=4)
                if h == 0:
                    self.P.add("act", lambda e, dst=dst, src=src: e.activation(out=dst, in_=src, func=AF.Copy),
                               [psr], [R["xT"][k] for k in range(h * 4, h * 4 + 4)])
                else:
                    self.cp(dst, src, [psr], [R["xT"][k] for k in range(h * 4, h * 4 + 4)])
                T.pspool.put((pst, psr))

    def store_tile(self, ti, pos, nb):
        T = self
        dr = T.dr
        R = T.R
        tot = NMETA + T.seq
        for b in range(nb):
            p0 = pos + b * 128
            lo = max(p0, NMETA)
            hi = min(p0 + 128, tot)
            if hi <= lo:
                continue
            sl = b % 2
            xr = R["xs"][sl]
            for h in range(2):
                pst, psr = T.pspool.get()
                for q in range(4):
                    kc = h * 4 + q
                    self.tr(pst[:, q * 128:(q + 1) * 128], T.xT[:, kc, b * 128:(b + 1) * 128], T.ident_f,
                            [R["xT"][kc], R["c"][0]], [psr])
                dst = T.xs[:, sl, h * 512:(h + 1) * 512]
                if h == 0:
                    self.P.add("act", lambda e, dst=dst, src=pst: e.activation(out=dst, in_=src[:, :], func=AF.Copy),
                               [psr], [xr])
                else:
                    self.cp(dst, pst[:, :], [psr], [xr])
                T.pspool.put((pst, psr))
            self.dma("sp", dr["out"][lo - NMETA:hi - NMETA, :], T.xs[lo - p0:hi - p0, sl, :], ("out", sl), [xr], [])

    def rmsnorm(self, l, gcol, nb):
        T = self
        R = T.R
        nt = nb * 128
        pst, psr = T.pspool.get()
        for kc in range(KC):
            bi, br = T.bpool.get()
            sq = T.scb[:, bi, 0:nt]
            self.act(sq, T.xT[:, kc, 0:nt], AF.Square, [R["xT"][kc]], [br])
            self.mm(pst[:, 0:nt], T.ones_b, sq, kc == 0, kc == KC - 1, [br, R["c"][0]], [psr])
            T.bpool.put((bi, br))
        fi, fr = T.fpool.get()
        rstd = T.scf[:, fi, 0:nt]
        self.rsqrt(rstd, pst[:, 0:nt], 1.0 / D, 1e-6, [psr], fr)
        T.pspool.put((pst, psr))
        for kc in range(KC):
            self.stt(T.hT[:, kc, 0:nt], T.xT[:, kc, 0:nt], T.pvt[:, l, gcol + kc:gcol + kc + 1], rstd, ALU.mult, ALU.mult,
                     [R["xT"][kc], fr, R["pv"][0]], [R["hT"][kc]])
        T.fpool.put((fi, fr))

    def ffn(self, l, which, nb):
        T = self
        dr = T.dr
        R = T.R
        nt = nb * 128
        pre = "ffn%d" % which
        w1 = dr[pre + "_w1"][l].rearrange("(k p) f -> p k f", p=128)
        w3 = dr[pre + "_w3"][l].rearrange("(k p) f -> p k f", p=128)
        w2 = dr[pre + "_w2"][l].rearrange("(c p) n -> p c n", p=128)
        self.rmsnorm(l, PV_F1G if which == 1 else PV_F2G, nb)
        hres = R["hT"]
        for half in range(2):
            for pi in range(6):
                nf = 2 if pi < 5 else 1
                f0 = (half * 11 + 2 * pi) * 128
                wdt = nf * 128

                def d1(base, wdt=wdt):
                    return base[:, 0:KC * wdt].rearrange("p (k f) -> p k f", k=KC)

                def d3(base, wdt=wdt):
                    return base[:, 2048:2048 + KC * wdt].rearrange("p (k f) -> p k f", k=KC)

                base, wres = self.piece([(d1, w1[:, :, f0:f0 + wdt]), (d3, w3[:, :, f0:f0 + wdt])])
                wa = d1(base)
                wb = d3(base)
                for j in range(nf):
                    fl = 2 * pi + j
                    pa, par = T.pspool.get()
                    pb, pbr = T.pspool.get()
                    for kc in range(KC):
                        self.mm(pa[:, 0:nt], wa[:, kc, j * 128:(j + 1) * 128], T.hT[:, kc, 0:nt], kc == 0, kc == KC - 1,
                                [wres, hres[kc]], [par])
                    for kc in range(KC):
                        self.mm(pb[:, 0:nt], wb[:, kc, j * 128:(j + 1) * 128], T.hT[:, kc, 0:nt], kc == 0, kc == KC - 1,
                                [wres, hres[kc]], [pbr])
                    fi, fr = T.fpool.get()
                    s = T.scf[:, fi, 0:nt]
                    self.sigmoid(s, pa[:, 0:nt], [par], fr)
                    self.tt(s, s, pa[:, 0:nt], ALU.mult, [fr, par], [fr])
                    self.tt(T.m[:, fl, 0:nt], s, pb[:, 0:nt], ALU.mult, [fr, pbr], [R["m"][fl]])
                    T.fpool.put((fi, fr))
                    T.pspool.put((pa, par))
                    T.pspool.put((pb, pbr))
            for pi in range(4):
                c0 = pi * 256

                def d2(base):
                    return base[:, 0:11 * 256].rearrange("p (c n) -> p c n", c=11)

                base, wres = self.piece([(d2, w2[:, half * 11:(half + 1) * 11, c0:c0 + 256])])
                ww = d2(base)
                for j in range(2):
                    dc = pi * 2 + j
                    py, pyr = T.pspool.get()
                    for fl in range(11):
                        self.mm(py[:, 0:nt], ww[:, fl, j * 128:(j + 1) * 128], T.m[:, fl, 0:nt], fl == 0, fl == 10,
                                [wres, R["m"][fl]], [pyr])
                    self.stt(T.xT[:, dc, 0:nt], py[:, 0:nt], 0.5, T.xT[:, dc, 0:nt], ALU.mult, ALU.add,
                             [pyr, R["xT"][dc]], [R["xT"][dc]])
                    T.pspool.put((py, pyr))

    def mixer(self, l, nb, first):
        T = self
        dr = T.dr
        R = T.R
        nt = nb * 128
        cres = R["c"][0]
        pvr = R["pv"][0]
        hres = R["hT"]
        win = dr["w_in"][l].rearrange("(k p) n -> p k n", p=128)
        self.rmsnorm(l, PV_MXG, nb)

        def std_piece(c0, wdt):
            def dd(base, wdt=wdt):
                return base[:, 0:KC * wdt].rearrange("p (k n) -> p k n", k=KC)
            base, wres = self.piece([(dd, win[:, :, c0:c0 + wdt])])
            return dd(base), wres

        def proj_fm(wv, wres, col0, out_ps, out_res):
            for kc in range(KC):
                self.mm(out_ps[:, 0:nt], wv[:, kc, col0:col0 + 128], T.hT[:, kc, 0:nt], kc == 0, kc == KC - 1,
                        [wres, hres[kc]], [out_res])

        def dq(base):
            return base[:, 0:4096].rearrange("p (k c g d) -> p k g c d", k=KC, c=4, g=2, d=64)
        srcq = win[:, :, 0:512].rearrange("p k (g c d) -> p k g c d", g=2, c=4, d=64)
        i, qwres = T.slots[T.slot_next % len(T.slots)]
        T.slot_next += 1
        qbase = T.ring[:, i, :]
        self.slot_dmas(i, qwres, [(dq(qbase)[:, :, g, c, :], srcq[:, :, g, c, :]) for g in range(2) for c in range(4)])
        wq = qbase[:, 0:4096].rearrange("p (k n) -> p k n", k=KC)
        wkv, kvres = std_piece(512, 256)

        def qknorm(src_ps, src_res, gcol, lnmul, dst, dst_res):
            bi, br = T.bpool.get()
            sq = T.scb[:, bi, 0:nt]
            self.act(sq, src_ps[:, 0:nt], AF.Square, [src_res], [br])
            p2, p2r = T.pspool.get()
            self.mm(p2[:, 0:nt], T.blk_b, sq, True, True, [br, cres], [p2r])
            T.bpool.put((bi, br))
            fi, fr = T.fpool.get()
            r = T.scf[:, fi, 0:nt]
            self.rsqrt(r, p2[:, 0:nt], 1.0, 64 * 1e-6, [p2r], fr, lnmul=lnmul)
            T.pspool.put((p2, p2r))
            self.stt(dst, src_ps[:, 0:nt], T.pvt[:, l, gcol:gcol + 1], r, ALU.mult, ALU.mult, [src_res, fr, pvr], [dst_res])
            T.fpool.put((fi, fr))

        for c in range(4):
            pq, pqr = T.pspool.get()
            proj_fm(wq, qwres, c * 128, pq, pqr)
            qknorm(pq, pqr, PV_GQ, 0.0, T.qn[:, c, 0:nt], R["qn"][c])
            T.pspool.put((pq, pqr))
        pk, pkr = T.pspool.get()
        proj_fm(wkv, kvres, 0, pk, pkr)
        qknorm(pk, pkr, PV_GK, math.log(8.0), T.kT[:, l, 128:128 + nt], R["kT"][l])
        T.pspool.put((pk, pkr))
        for b in range(nb):
            pv_, pvr_ = T.pspool.get()
            for kc in range(KC):
                self.mm(pv_[:, 0:128], T.hT[:, kc, b * 128:(b + 1) * 128], wkv[:, kc, 128:256], kc == 0, kc == KC - 1,
                        [kvres, hres[kc]], [pvr_])
            self.cp(T.vtk[:, l, 1 + b, :], pv_[:, 0:128], [pvr_], [R["vtk"][l]])
            T.pspool.put((pv_, pvr_))
        ktr = R["kT"][l]
        vtr = R["vtk"][l]
        for qb in range(nb):
            has_prev = not (first and qb == 0)
            for g in range(2):
                gs_ = slice(g * 64, (g + 1) * 64)
                rhs_q = T.qn[gs_, :, qb * 128:(qb + 1) * 128]
                blocks = [(1, T.mown4)]
                if has_prev:
                    blocks.append((0, T.mprev4))
                ptiles = []
                for (own, mask) in blocks:
                    kcol = (128 + qb * 128) if own else (qb * 128)
                    pss, pssr = T.pspool.get()
                    self.mm(pss[:, :], T.kT[gs_, l, kcol:kcol + 128], rhs_q, True, True, [ktr] + R["qn"], [pssr])
                    bi, br = T.bpool.get()
                    pt = T.scb[:, bi, 0:512]
                    self.act(pt, pss[:, :], AF.Exp, [pssr], [br])
                    T.pspool.put((pss, pssr))
                    self.tt(pt, pt, mask, ALU.mult, [br, cres], [br])
                    ptiles.append((own, pt, bi, br))
                po, por = T.pspool.get()
                pd, pdr = T.pspool.get()
                n = len(ptiles)
                for ii, (own, pt, bi, br) in enumerate(ptiles):
                    vb = (1 + qb) if own else qb
                    self.mm(po[:, :], T.vtk[:, l, vb, :], pt, ii == 0, ii == n - 1, [vtr, br], [por])
                for ii, (own, pt, bi, br) in enumerate(ptiles):
                    self.mm(pd[:, :], T.ones_b, pt, ii == 0, ii == n - 1, [cres, br], [pdr])
                for (own, pt, bi, br) in ptiles:
                    T.bpool.put((bi, br))
                fi, fr = T.fpool.get()
                dt_ = T.scf[gs_, fi, 0:512]
                self.tt(dt_, pd[gs_, :], T.sinkx[gs_, l, :], ALU.add, [pdr, pvr], [fr])
                self.recip(dt_, dt_, [fr], [fr])
                T.pspool.put((pd, pdr))
                self.tt(T.oa[gs_, :, qb * 128:(qb + 1) * 128], po[gs_, :].rearrange("p (j q) -> p j q", j=4),
                        dt_.rearrange("p (j q) -> p j q", j=4), ALU.mult, [por, fr], R["oa"])
                T.fpool.put((fi, fr))
                T.pspool.put((po, por))
        self.act(T.kT[:, l, 0:128], T.kT[:, l, nt:nt + 128], AF.Copy, [ktr], [ktr])
        self.act(T.vtk[:, l, 0, :], T.vtk[:, l, nb, :], AF.Copy, [vtr], [vtr])

        if STOP == "attn":
            self.stopped = True
            return
        for pi in range(2):
            def da(base):
                return base[:, 0:2048].rearrange("p (k n) -> p k n", k=KC)

            def dg(base):
                return base[:, 2048:4096].rearrange("p (k n) -> p k n", k=KC)
            base, wres = self.piece([(da, win[:, :, 768 + pi * 256:768 + pi * 256 + 256]),
                                     (dg, win[:, :, 1280 + pi * 256:1280 + pi * 256 + 256])])
            wa, wg = da(base), dg(base)
            for j in range(2):
                c = pi * 2 + j
                pa, par = T.pspool.get()
                pg, pgr = T.pspool.get()
                proj_fm(wa, wres, j * 128, pa, par)
                proj_fm(wg, wres, j * 128, pg, pgr)
                fi, fr = T.fpool.get()
                s = T.scf[:, fi, 0:nt]
                self.sigmoid(s, pg[:, 0:nt], [pgr], fr)
                T.pspool.put((pg, pgr))
                self.tt(T.u[:, l, c, 30:30 + nt], s, pa[:, 0:nt], ALU.mult, [fr, par], [R["u"][l * 4 + c]])
                T.fpool.put((fi, fr))
                T.pspool.put((pa, par))
        for c in range(4):
            ur = R["u"][l * 4 + c]
            ar = R["acc"][c]
            wc = PV_CW + c * 31
            self.ts(T.acc[:, c, 0:nt], T.u[:, l, c, 0:nt], T.pvt[:, l, wc:wc + 1], T.pvt[:, l, PV_CB + c:PV_CB + c + 1],
                    ALU.mult, ALU.add, [ur, pvr], [ar])
        for j in range(1, 31):
            for c in range(4):
                ur = R["u"][l * 4 + c]
                ar = R["acc"][c]
                wc = PV_CW + c * 31 + j
                self.stt(T.acc[:, c, 0:nt], T.u[:, l, c, j:j + nt], T.pvt[:, l, wc:wc + 1], T.acc[:, c, 0:nt],
                         ALU.mult, ALU.add, [ur, pvr, ar], [ar])
        for c in range(4):
            ur = R["u"][l * 4 + c]
            self.act(T.u[:, l, c, 0:30], T.u[:, l, c, nt:nt + 30], AF.Copy, [ur], [ur])
        pm, pmr = T.pspool.get()
        for c in range(4):
            bi, br = T.bpool.get()
            ab = T.scb[:, bi, 0:nt]
            self.act(ab, T.acc[:, c, 0:nt], AF.Copy, [R["acc"][c]], [br])
            self.mm(pm[:, 0:nt], T.ones_b, ab, c == 0, c == 3, [br, cres], [pmr])
            T.bpool.put((bi, br))
        for c in range(4):
            self.stt(T.acc[:, c, 0:nt], pm[:, 0:nt], -1.0 / 512, T.acc[:, c, 0:nt], ALU.mult, ALU.add,
                     [pmr, R["acc"][c]], [R["acc"][c]])
        T.pspool.put((pm, pmr))
        pv2, pv2r = T.pspool.get()
        for c in range(4):
            bi, br = T.bpool.get()
            sq = T.scb[:, bi, 0:nt]
            self.act(sq, T.acc[:, c, 0:nt], AF.Square, [R["acc"][c]], [br])
            self.mm(pv2[:, 0:nt], T.ones_b, sq, c == 0, c == 3, [br, cres], [pv2r])
            T.bpool.put((bi, br))
        fi, fr = T.fpool.get()
        rstd = T.scf[:, fi, 0:nt]
        self.rsqrt(rstd, pv2[:, 0:nt], 1.0 / 512, 1e-5, [pv2r], fr)
        T.pspool.put((pv2, pv2r))
        for c in range(4):
            ar = R["acc"][c]
            self.tt(T.acc[:, c, 0:nt], T.acc[:, c, 0:nt], rstd, ALU.mult, [ar, fr], [ar])
            self.act(T.acc[:, c, 0:nt], T.acc[:, c, 0:nt], AF.Identity, [ar, pvr], [ar],
                     scale=T.pvt[:, l, PV_LG + c:PV_LG + c + 1], bias=T.pvt[:, l, PV_LB + c:PV_LB + c + 1])
            f2, f2r = T.fpool.get()
            s = T.scf[:, f2, 0:nt]
            self.sigmoid(s, T.acc[:, c, 0:nt], [ar], f2r)
            self.tt(T.cb[:, c, 0:nt], s, T.acc[:, c, 0:nt], ALU.mult, [f2r, ar], [R["cb"][c]])
            T.fpool.put((f2, f2r))
        T.fpool.put((fi, fr))

        if STOP == "conv":
            self.stopped = True
            return
        hist = R["qkh"][l]
        for qk in range(2):
            wv, wres = std_piece(1792 + qk * 512, 512)
            for c in range(4):
                ch = qk * 4 + c
                pq, pqr = T.pspool.get()
                proj_fm(wv, wres, c * 128, pq, pqr)
                rs = ch % 2
                rr = R["raw"][rs]
                self.P.add("act", lambda e, o=T.raw[:, rs, 3:3 + nt], i_=pq[:, 0:nt]: e.activation(out=o, in_=i_, func=AF.Copy),
                           [pqr], [rr])
                T.pspool.put((pq, pqr))
                self.act(T.raw[:, rs, 0:3], T.qkh[:, l, ch, :], AF.Copy, [hist], [rr])
                car = R["cacc"][rs]
                wc = PV_QW + ch * 4
                self.ts(T.cacc[:, rs, 0:nt], T.raw[:, rs, 0:nt], T.pvt[:, l, wc:wc + 1], T.pvt[:, l, PV_QB + ch:PV_QB + ch + 1],
                        ALU.mult, ALU.add, [rr, pvr], [car])
                for j in range(1, 4):
                    self.stt(T.cacc[:, rs, 0:nt], T.raw[:, rs, j:j + nt], T.pvt[:, l, wc + j:wc + j + 1], T.cacc[:, rs, 0:nt],
                             ALU.mult, ALU.add, [rr, pvr, car], [car])
                self.act(T.qkh[:, l, ch, :], T.raw[:, rs, nt:nt + 3], AF.Copy, [rr], [hist])
                fi, fr = T.fpool.get()
                s = T.scf[:, fi, 0:nt]
                self.sigmoid(s, T.cacc[:, rs, 0:nt], [car], fr)
                self.tt(T.mqk[:, ch, 0:nt], s, T.cacc[:, rs, 0:nt], ALU.mult, [fr, car], [R["mqk"][ch]])
                T.fpool.put((fi, fr))
        for b in range(nb):
            pst, psr = T.pspool.get()
            pstb = pst.bitcast(BF16)
            for h in range(4):
                self.tr(pstb[:, h * 128:(h + 1) * 128], T.mqk[:, 4 + h, b * 128:(b + 1) * 128], T.ident_b,
                        [R["mqk"][4 + h], cres], [psr])
            self.cp(T.ktok[:, b, :, :], pstb[:, 0:512].rearrange("p (h d) -> p h d", h=4), [psr], [R["ktok"][b]])
            T.pspool.put((pst, psr))
        wv, wres = std_piece(2816, 512)
        for b in range(nb):
            pv_, pvr_ = T.pspool.get()
            for kc in range(KC):
                self.mm(pv_[:, :], T.hT[:, kc, b * 128:(b + 1) * 128], wv[:, kc, 0:512], kc == 0, kc == KC - 1,
                        [wres, hres[kc]], [pvr_])
            self.P.add("act", lambda e, o=T.vaug[:, b, :, 0:128], i_=pv_[:, :].rearrange("p (h d) -> p h d", h=4):
                       e.activation(out=o, in_=i_, func=AF.Copy), [pvr_], [R["vaug"][b]])
            T.pspool.put((pv_, pvr_))
        wv, wres = std_piece(3328, 512)
        for c in range(4):
            po, por = T.pspool.get()
            proj_fm(wv, wres, c * 128, po, por)
            fi, fr = T.fpool.get()
            s = T.scf[:, fi, 0:nt]
            self.sigmoid(s, po[:, 0:nt], [por], fr)
            T.pspool.put((po, por))
            self.cp(T.so[:, c, 0:nt], s, [fr], [R["so"][c]])
            T.fpool.put((fi, fr))
        if STOP == "m3":
            self.stopped = True
            return
        def dgt(base):
            return base[:, 0:1024].rearrange("p (k n) -> p k n", k=KC)
        gbase, gwres = self.piece([(dgt, win[:, :, 3840:3968])])
        wgt = dgt(gbase)
        Cr, Cbr, nbr = R["Cst"][l], R["Cbf"][l], R["nbc"][l]
        isq = 1.0 / math.sqrt(128.0)
        for b in range(nb):
            cs = slice(b * 128, (b + 1) * 128)
            smi = b % 2
            smr = R["sm"][smi]
            sm = T.sm[:, smi * 4:(smi + 1) * 4, :]
            pg, pgr = T.pspool.get()
            for kc in range(KC):
                self.mm(pg[:, 0:16], T.hT[:, kc, cs], wgt[:, kc, 0:16], kc == 0, kc == KC - 1, [gwres, hres[kc]], [pgr])
            self.tt(sm[:, 0, :], pg[:, 0:8], T.pvt[:, l, PV_GT:PV_GT + 8], ALU.add, [pgr, pvr], [smr])
            T.pspool.put((pg, pgr))
            if STOP == "g1":
                continue
            self.act(sm[:, 1, 0:4], sm[:, 0, 4:8], AF.Exp, [smr], [smr], scale=-1.0)
            self.act(sm[:, 1, 0:4], sm[:, 1, 0:4], AF.Ln, [smr], [smr], bias=1.0)
            if STOP == "g2":
                continue
            fi, fr = T.fpool.get()
            nlb = T.scf[:, fi, 0:512]
            for h in range(4):
                self.ts(nlb[:, h * 128:(h + 1) * 128], T.ones_f, sm[:, 1, h:h + 1], None, ALU.mult, None, [cres, smr], [fr])
            pnb, pnbr = T.pspool.get()
            for h in range(4):
                self.mm(pnb[:, h * 128:(h + 1) * 128], nlb[:, h * 128:(h + 1) * 128], T.tri_f, True, True, [fr, cres], [pnbr])
            self.tt(nlb, pnb[:, :], T.ident4_f, ALU.mult, [pnbr, cres], [fr])
            self.P.add("dve", lambda e, o=sm[:, 1, 4:8], i_=nlb.rearrange("p (h t) -> p h t", h=4):
                       e.reduce_sum(out=o, in_=i_, axis=mybir.AxisListType.X), [fr], [smr])
            self.tt(sm[:, 1, 4:8], sm[:, 1, 4:8], sm[:, 0, 0:4], ALU.add, [smr], [smr])
            T.fpool.put((fi, fr))
            if STOP == "g3":
                T.pspool.put((pnb, pnbr))
                continue
            fd, fdr = T.fpool.get()
            Dm = T.scf[:, fd, 0:512]
            self.tt(Dm, T.maskb4, pnb[:, :], ALU.subtract, [cres, pnbr], [fdr])
            for h in range(4):
                self.act(Dm[:, h * 128:(h + 1) * 128], Dm[:, h * 128:(h + 1) * 128], AF.Exp, [fdr, smr], [fdr],
                         bias=sm[:, 1, 4 + h:5 + h])
            if STOP == "g4a":
                T.pspool.put((pnb, pnbr)); T.fpool.put((fd, fdr))
                continue
            fe, fer = T.fpool.get()
            eb = T.scf[:, fe, 0:512]
            self.act(eb, pnb[:, :], AF.Exp, [pnbr], [fer], scale=-1.0)
            if STOP == "g4b":
                T.pspool.put((pnb, pnbr)); T.fpool.put((fd, fdr)); T.fpool.put((fe, fer))
                continue
            T.pspool.put((pnb, pnbr))
            self.act(sm[:, 2, 0:4], sm[:, 1, 4:8], AF.Exp, [smr], [smr])
            ebv = eb.rearrange("p (h t) -> p h t", h=4)
            self.tt(sm[:, 3, 0:4], sm[:, 2, 0:4], ebv[:, :, 127], ALU.mult, [smr, fer], [smr])
            if STOP == "m4":
                T.fpool.put((fe, fer)); T.fpool.put((fd, fdr))
                continue
            bq, bqr = T.bpool.get()
            qs = T.scb[:, bq, 0:512]
            self.stt(qs.rearrange("p (h t) -> p h t", h=4), T.mqk[:, 0:4, cs], isq, eb.rearrange("p (h t) -> p h t", h=4),
                     ALU.mult, ALU.mult, R["mqk"][0:4] + [fer], [bqr])
            pgm, pgmr = T.pspool.get()
            for h in range(4):
                self.mm(pgm[:, h * 128:(h + 1) * 128], T.mqk[:, 4 + h, cs], T.mqk[:, h, cs], True, True,
                        [R["mqk"][4 + h], R["mqk"][h]], [pgmr])
            bp, bpr = T.bpool.get()
            Pm = T.scb[:, bp, 0:512]
            self.stt(Pm, pgm[:, :], isq, Dm, ALU.mult, ALU.mult, [pgmr, fdr], [bpr])
            T.pspool.put((pgm, pgmr))
            T.fpool.put((fd, fdr))
            pn, pnr = T.pspool.get()
            pd, pdr = T.pspool.get()
            for h in range(4):
                hs = slice(h * 128, (h + 1) * 128)
                self.mm(pn[:, hs], T.vaug[:, b, h, 0:128], Pm[:, hs], True, False, [R["vaug"][b], bpr], [pnr])
                self.mm(pn[:, hs], T.Cbf[:, l, h, :], qs[:, hs], False, True, [Cbr, bqr], [pnr])
            for h in range(4):
                hs = slice(h * 128, (h + 1) * 128)
                self.mm(pd[:, hs], T.ones_b, Pm[:, hs], True, False, [cres, bpr], [pdr])
                self.mm(pd[:, hs], T.nbc[:, l, h, :], qs[:, hs], False, True, [nbr, bqr], [pdr])
            T.bpool.put((bp, bpr))
            T.bpool.put((bq, bqr))
            fn_, fnr = T.fpool.get()
            dn = T.scf[:, fn_, 0:512]
            self.act(dn, pd[:, :], AF.Abs, [pdr], [fnr])
            T.pspool.put((pd, pdr))
            self.ts(dn, dn, 1.0, None, ALU.max, None, [fnr], [fnr])
            self.recip(dn, dn, [fnr], [fnr])
            self.tt(dn, pn[:, :], dn, ALU.mult, [pnr, fnr], [fnr])
            T.pspool.put((pn, pnr))
            self.tt(T.hm[:, :, cs], dn.rearrange("p (h t) -> p h t", h=4), T.so[:, :, cs], ALU.mult, [fnr] + R["so"], R["hm"])
            T.fpool.put((fn_, fnr))
            bv, bvr = T.bpool.get()
            vw = T.scb[:, bv, 0:520].rearrange("p (h e) -> p h e", h=4)
            for h in range(4):
                self.ts(vw[:, h, 0:129], T.vaug[:, b, h, 0:129], sm[:, 3, h:h + 1], None, ALU.mult, None, [R["vaug"][b], smr], [bvr])
            pc0, pc0r = T.pspool.get()
            pc1, pc1r = T.pspool.get()
            for h in range(4):
                pcc, pccr = (pc0, pc0r) if h < 2 else (pc1, pc1r)
                o = (h % 2) * 130
                self.mm(pcc[:, o:o + 129], T.ktok[:, b, h, :], vw[:, h, 0:129], True, True, [R["ktok"][b], bvr], [pccr])
            T.bpool.put((bv, bvr))
            for h in range(4):
                pcc, pccr = (pc0, pc0r) if h < 2 else (pc1, pc1r)
                o = (h % 2) * 130
                self.stt(T.Cst[:, l, h, :], T.Cst[:, l, h, :], eb[:, h * 128 + 127:h * 128 + 128], pcc[:, o:o + 129],
                         ALU.mult, ALU.add, [Cr, fer, pccr], [Cr])
            T.pspool.put((pc0, pc0r))
            T.pspool.put((pc1, pc1r))
            T.fpool.put((fe, fer))
            self.P.add("act", lambda e, o=T.Cbf[:, l, :, :], i_=T.Cst[:, l, :, 0:128]: e.activation(out=o, in_=i_, func=AF.Copy),
                       [Cr], [Cbr])
            for h in range(4):
                self.act(T.nbc[:, l, h, :], T.ones_f, AF.Identity, [cres, Cr], [nbr], scale=T.Cst[:, l, h, 128:129])

        if STOP in ("mlstm", "m4", "g1", "g2", "g3", "g4a", "g4b"):
            self.stopped = True
            return
        woa = dr["w_o_attn"][l]
        woc = dr["w_o_conv"][l].rearrange("(j p) n -> p j n", p=128)
        wom = dr["w_o_mlstm"][l].rearrange("(j p) n -> p j n", p=128)
        zg = win[:, :, 3848:6920].rearrange("p k (b n) -> p b k n", b=3)
        for dc in range(8):
            ds_ = slice(dc * 128, (dc + 1) * 128)

            def dz(base):
                return base[:, 0:3072].rearrange("p (b k n) -> p b k n", b=3, k=KC)
            iz, zres = T.slots[T.slot_next % len(T.slots)]
            T.slot_next += 1
            zbase = T.ring[:, iz, :]
            wz = dz(zbase)
            self.slot_dmas(iz, zres, [(wz[:, b_, :, :], zg[:, b_, :, ds_]) for b_ in range(3)])
            i2, ores = T.slots[T.slot_next % len(T.slots)]
            T.slot_next += 1
            obase = T.ring[:, i2, :]
            wo = obase[:, 0:1536].rearrange("p (b j n) -> p b j n", b=3, j=4)
            prs = []
            for g in range(2):
                src = woa[g * 256:(g + 1) * 256, ds_].rearrange("(j d) n -> d j n", d=64)
                prs.append((wo[g * 64:(g + 1) * 64, 0, :, :], src))
            prs.append((wo[:, 1, :, :], woc[:, :, ds_]))
            prs.append((wo[:, 2, :, :], wom[:, :, ds_]))
            self.slot_dmas(i2, ores, prs)
            gts = []
            for br_ in range(3):
                pz, pzr = T.pspool.get()
                for kc in range(KC):
                    self.mm(pz[:, 0:nt], wz[:, br_, kc, :], T.hT[:, kc, 0:nt], kc == 0, kc == KC - 1, [zres, hres[kc]], [pzr])
                fi, fr = T.fpool.get()
                s = T.scf[:, fi, 0:nt]
                self.sigmoid(s, pz[:, 0:nt], [pzr, pvr], fr, negbias=T.ngb[:, l, br_ * 8 + dc:br_ * 8 + dc + 1])
                T.pspool.put((pz, pzr))
                gts.append((s, fi, fr))
            srcs = [(T.oa, R["oa"]), (T.cb, R["cb"]), (T.hm, R["hm"])]
            for br_ in range(3):
                py, pyr = T.pspool.get()
                buf, bres = srcs[br_]
                for j in range(4):
                    self.mm(py[:, 0:nt], wo[:, br_, j, :], buf[:, j, 0:nt], j == 0, j == 3, [ores, bres[j]], [pyr])
                s, fi, fr = gts[br_]
                self.tt(s, s, py[:, 0:nt], ALU.mult, [fr, pyr], [fr])
                T.pspool.put((py, pyr))
            s0, f0, r0 = gts[0]
            s1, f1, r1 = gts[1]
            s2, f2, r2 = gts[2]
            self.tt(s0, s0, s1, ALU.add, [r0, r1], [r0])
            self.tt(T.mg[:, dc, 0:nt], s0, s2, ALU.add, [r0, r2], [R["mg"][dc]])
            for (s, fi, fr) in gts:
                T.fpool.put((fi, fr))
        wout = dr["w_out"][l].rearrange("(k p) n -> p k n", p=128)
        for pi in range(2):
            def dd(base):
                return base[:, 0:4096].rearrange("p (k n) -> p k n", k=KC)
            base, wres = self.piece([(dd, wout[:, :, pi * 512:(pi + 1) * 512])])
            wv = dd(base)
            for j in range(4):
                dc = pi * 4 + j
                py, pyr = T.pspool.get()
                for kc in range(KC):
                    self.mm(py[:, 0:nt], wv[:, kc, j * 128:(j + 1) * 128], T.mg[:, kc, 0:nt], kc == 0, kc == KC - 1,
                            [wres, R["mg"][kc]], [pyr])
                self.tt(T.xT[:, dc, 0:nt], py[:, 0:nt], T.xT[:, dc, 0:nt], ALU.add, [pyr, R["xT"][dc]], [R["xT"][dc]])
                T.pspool.put((py, pyr))


def make_consts():
    c32 = np.zeros((128, 1408), np.float32)
    r = np.arange(128)
    c32[:, 0:128] = np.eye(128, dtype=np.float32)
    tri = (r[:, None] <= r[None, :]).astype(np.float32)
    c32[:, 128:256] = tri
    maskb = np.where(r[:, None] <= r[None, :], 0.0, -30000.0).astype(np.float32)
    c32[:, 256:768] = np.tile(maskb, (1, 4))
    c32[:, 768:896] = 1.0
    c32[:, 896:1408] = np.tile(np.eye(128, dtype=np.float32), (1, 4))
    cbf = np.zeros((128, 1408), np.float32)
    cbf[:, 0:128] = 1.0
    blk = np.zeros((128, 128), np.float32)
    blk[0:64, 0:64] = 1.0
    blk[64:128, 64:128] = 1.0
    cbf[:, 128:256] = blk
    cbf[:, 256:384] = np.eye(128, dtype=np.float32)
    cbf[:, 384:896] = np.tile(tri, (1, 4))
    cbf[:, 896:1408] = np.tile(1.0 - tri, (1, 4))
    return c32, cbf


def make_pv(inp):
    L = DEPTH
    pv = np.zeros((L, 128, NV), np.float32)
    for l in range(L):
        def fm(v, n):
            return np.ascontiguousarray(np.asarray(v, np.float32).reshape(n, 128).T)
        pv[l, :, PV_F1G:PV_F1G + 8] = fm(inp["ffn1_norm"][l], 8)
        pv[l, :, PV_MXG:PV_MXG + 8] = fm(inp["mix_norm"][l], 8)
        pv[l, :, PV_F2G:PV_F2G + 8] = fm(inp["ffn2_norm"][l], 8)
        gb = np.asarray(inp["gate_bias"][l], np.float32)
        for b in range(3):
            pv[l, :, PV_GB + b * 8:PV_GB + b * 8 + 8] = fm(gb[b], 8)
        cw = np.asarray(inp["conv_dw_w"][l], np.float32)
        for c in range(4):
            pv[l, :, PV_CW + c * 31:PV_CW + (c + 1) * 31] = cw[:, c * 128:(c + 1) * 128].T
        pv[l, :, PV_CB:PV_CB + 4] = fm(inp["conv_dw_b"][l], 4)
        pv[l, :, PV_LG:PV_LG + 4] = fm(inp["conv_ln_g"][l], 4)
        pv[l, :, PV_LB:PV_LB + 4] = fm(inp["conv_ln_b"][l], 4)
        qw = np.asarray(inp["mlstm_qk_conv_w"][l], np.float32)
        for c in range(8):
            pv[l, :, PV_QW + c * 4:PV_QW + (c + 1) * 4] = qw[:, c * 128:(c + 1) * 128].T
        pv[l, :, PV_QB:PV_QB + 8] = fm(inp["mlstm_qk_conv_b"][l], 8)
        pv[l, :, PV_GQ] = np.tile(np.asarray(inp["attn_q_norm"][l], np.float32), 2)
        pv[l, :, PV_GK] = np.tile(np.asarray(inp["attn_k_norm"][l], np.float32), 2)
        sk = np.asarray(inp["attn_sinks"][l], np.float32)
        pv[l, 0:64, PV_SK:PV_SK + 4] = sk[None, 0:4]
        pv[l, 64:128, PV_SK:PV_SK + 4] = sk[None, 4:8]
        pv[l, :, PV_GT:PV_GT + 4] = np.asarray(inp["mlstm_igate_bias"][l], np.float32)[None, :]
        pv[l, :, PV_GT + 4:PV_GT + 8] = np.asarray(inp["mlstm_fgate_bias"][l], np.float32)[None, :]
    return pv


_NC_CACHE = {}


def run(inputs, n_cores=None, depth=DEPTH, trace=False):
    x = np.asarray(inputs["x"], np.float32)
    bsz, seq, _ = x.shape
    if n_cores is None:
        n_cores = bsz
    key = (seq, depth, STOP)
    if key not in _NC_CACHE:
        _NC_CACHE[key] = Builder(seq, depth).build()
    nc = _NC_CACHE[key]
    c32, cbf = make_consts()
    pv = make_pv(inputs)
    shared = {"meta": np.ascontiguousarray(np.asarray(inputs["meta_tokens"], np.float32)), "pv": pv, "c32": c32, "cbf": cbf}
    for k in ("ffn1_w1", "ffn1_w3", "ffn1_w2", "ffn2_w1", "ffn2_w3", "ffn2_w2", "w_in", "w_o_attn", "w_o_conv",
              "w_o_mlstm", "w_out"):
        shared[k] = np.ascontiguousarray(np.asarray(inputs[k], np.float32))
    in_maps = []
    for b in range(n_cores):
        d = dict(shared)
        d["xin"] = np.ascontiguousarray(x[b])
        in_maps.append(d)
    res = run_bass_kernel_spmd(nc, in_maps, core_ids=list(range(n_cores)), **({"trace": True} if trace else {}))
    out = np.stack([np.asarray(r["out"], np.float32) for r in res.results], axis=0)
    return out, res


def kernel(**inputs):
    out, _ = run(inputs)
    return out
```

```python
import math
from collections import deque
from contextlib import ExitStack

import numpy as np
import concourse.bass as bass
import concourse.mybir as mybir
from concourse.bass_utils import run_bass_kernel_spmd

F32 = mybir.dt.float32
BF16 = mybir.dt.bfloat16
AF = mybir.ActivationFunctionType
ALU = mybir.AluOpType

D = 1024
KC = 8
DFF = 2816
NIN = 6920
NMETA = 16
DEPTH = 2
ENGS = ("pe", "act", "dve", "pool", "sp")
STOP = None
POOL_CHUNKS = ()
NSW = 40

PV_F1G, PV_MXG, PV_F2G, PV_GB, PV_CW, PV_CB, PV_LG, PV_LB, PV_QW, PV_QB, PV_GQ, PV_GK, PV_SK, PV_GT, NV = (
    0, 8, 16, 24, 48, 172, 176, 180, 184, 216, 224, 225, 226, 230, 238)


class Res:
    __slots__ = ("lw", "rd")

    def __init__(self):
        self.lw = []
        self.rd = {}


def mkres(n):
    return [Res() for _ in range(n)]


class Prog:
    def __init__(self):
        self.st = {e: [] for e in ENGS}
        self.seen = {e: {} for e in ENGS}
        self.dmacnt = {}
        self.nsw = 0

    def add(self, eng, fn, reads=(), writes=(), dmakey=None, lw_only=()):
        st = self.st[eng]
        idx = len(st)
        need = {}
        isdma = dmakey is not None
        if dmakey is None:
            tok = ("e", eng, idx)
        else:
            if eng == "pool":
                dmakey = ("sw", self.nsw % NSW)
                self.nsw += 1
                prev = self.dmacnt.get(dmakey, 0)
                if prev:
                    need[("d", dmakey)] = prev
            c = self.dmacnt.get(dmakey, 0) + 1
            self.dmacnt[dmakey] = c
            tok = ("d", dmakey, c)

        def want(t, raw):
            if t is None:
                return
            kind, key, val = t
            if kind == "e" and key == eng and not isdma:
                if (not raw) or eng == "pe":
                    return
            k = (kind, key)
            if need.get(k, -1) < val:
                need[k] = val

        for r in reads:
            for t_ in r.lw:
                want(t_, True)
        for w in writes:
            for t_ in w.lw:
                want(t_, False)
            for k, v in w.rd.items():
                want((k[0], k[1], v), False)
        waits = []
        sn = self.seen[eng]
        for k, v in need.items():
            if sn.get(k, -1) < v:
                sn[k] = v
                waits.append((k[0], k[1], v))
        st.append((fn, waits, dmakey))
        k = (tok[0], tok[1])
        for r in reads:
            if r.rd.get(k, -1) < tok[2]:
                r.rd[k] = tok[2]
        for w in writes:
            w.lw = [tok]
            w.rd = {}
        for w in lw_only:
            w.lw.append(tok)
        return tok

    def emit(self, nc, es, final_dma_keys):
        ranks = {e: {} for e in ENGS}
        used = {e: set() for e in ENGS}
        for e in ENGS:
            for (_, waits, _) in self.st[e]:
                for kind, key, val in waits:
                    if kind == "e":
                        used[key].add(val)
        for e in ENGS:
            for i, idx in enumerate(sorted(used[e])):
                ranks[e][idx] = i + 1
        esem = {e: es.enter_context(nc.semaphore("sem_" + e)) for e in ENGS}
        dsem = {k: es.enter_context(nc.semaphore("dsem%d" % n)) for n, k in enumerate(self.dmacnt)}
        block = es.enter_context(nc.Block())
        st = self.st
        dmacnt = self.dmacnt

        def run(e, eng):
            rk = ranks[eng]
            for i, (fn, waits, dmakey) in enumerate(st[eng]):
                for kind, key, val in waits:
                    if kind == "e":
                        e.wait_ge(esem[key], ranks[key][val])
                    else:
                        e.wait_ge(dsem[key], 16 * val)
                ins = fn(e)
                if dmakey is not None:
                    ins.then_inc(dsem[dmakey], 16)
                elif i in rk:
                    ins.then_inc(esem[eng], 1)
            if eng == "sp":
                for k in final_dma_keys:
                    if k in dmacnt:
                        e.wait_ge(dsem[k], 16 * dmacnt[k])

        @block.tensor
        def _(e):
            run(e, "pe")

        @block.scalar
        def _(e):
            run(e, "act")

        @block.vector
        def _(e):
            run(e, "dve")

        @block.gpsimd
        def _(e):
            run(e, "pool")

        @block.sync
        def _(e):
            run(e, "sp")


class Pool:
    def __init__(self, items):
        self.free = deque(items)

    def get(self):
        assert self.free, "scratch pool exhausted"
        return self.free.popleft()

    def put(self, it):
        self.free.append(it)


class Builder:
    def __init__(self, seq, depth=DEPTH, nt_max=512):
        self.seq = seq
        self.depth = depth
        tot = NMETA + seq
        self.tiles = []
        pos = 0
        while pos < tot:
            nb = min(nt_max // 128, (tot - pos + 127) // 128)
            self.tiles.append((pos, nb))
            pos += nb * 128
        self.P = Prog()
        self.dq = deque()

    def defer(self, fn):
        self.dq.append(fn)

    def pump(self, n=None):
        k = 0
        while self.dq and (n is None or k < n):
            self.dq.popleft()()
            k += 1

    def mm(self, out, lhsT, rhs, start, stop, reads, writes):
        self.P.add("pe", lambda e: e.matmul(out, lhsT=lhsT, rhs=rhs, start=start, stop=stop), reads, writes)

    def tr(self, out, in_, ident, reads, writes):
        self.P.add("pe", lambda e: e.transpose(out, in_, ident), reads, writes)

    def act(self, out, in_, func, reads, writes, bias=None, scale=None):
        kw = {}
        if bias is not None:
            kw["bias"] = bias
        if scale is not None:
            kw["scale"] = scale
        self.P.add("act", lambda e: e.activation(out=out, in_=in_, func=func, **kw), reads, writes)

    def tt(self, out, in0, in1, op, reads, writes, eng="dve"):
        self.P.add(eng, lambda e: e.tensor_tensor(out=out, in0=in0, in1=in1, op=op), reads, writes)

    def ts(self, out, in0, s1, s2, op0, op1, reads, writes, eng="dve"):
        if s2 is None:
            self.P.add(eng, lambda e: e.tensor_scalar(out=out, in0=in0, scalar1=s1, scalar2=None, op0=op0), reads, writes)
        else:
            self.P.add(eng, lambda e: e.tensor_scalar(out=out, in0=in0, scalar1=s1, scalar2=s2, op0=op0, op1=op1), reads, writes)

    def stt(self, out, in0, scalar, in1, op0, op1, reads, writes, eng="dve"):
        self.P.add(eng, lambda e: e.scalar_tensor_tensor(out=out, in0=in0, scalar=scalar, in1=in1, op0=op0, op1=op1), reads, writes)

    def cp(self, out, in_, reads, writes, eng="dve"):
        self.P.add(eng, lambda e: e.tensor_copy(out=out, in_=in_), reads, writes)

    def recip(self, out, in_, reads, writes):
        self.P.add("dve", lambda e: e.reciprocal(out=out, in_=in_), reads, writes)

    def memset(self, ap, val, writes, eng="dve"):
        self.P.add(eng, lambda e: e.memset(ap, val), (), writes)

    def dma(self, eng, out, in_, key, reads, writes, lw_only=()):
        self.P.add(eng, lambda e: e.dma_start(out=out, in_=in_), reads, writes, dmakey=key, lw_only=lw_only)

    def slot_dmas(self, i, res, pairs):
        for n, (dst, src) in enumerate(pairs):
            if n == 0:
                self.dma("pool", dst, src, ("w", i), [], [res])
            else:
                self.dma("pool", dst, src, ("w", i), [], [], lw_only=[res])

    def sigmoid(self, out, in_, reads, wres, negbias=None):
        self.act(out, in_, AF.Exp, reads, [wres], scale=-1.0, bias=negbias)
        self.act(out, out, AF.Ln, [wres], [wres], bias=1.0)
        self.act(out, out, AF.Exp, [wres], [wres], scale=-1.0)

    def rsqrt(self, out, in_, scale, eps, reads, wres, lnmul=0.0):
        self.act(out, in_, AF.Ln, reads, [wres], scale=scale, bias=eps)
        if lnmul != 0.0:
            self.act(out, out, AF.Exp, [wres], [wres], scale=-0.5, bias=lnmul)
        else:
            self.act(out, out, AF.Exp, [wres], [wres], scale=-0.5)

    def build(self):
        nc = bass.Bass("TRN2", target_bir_lowering=False)
        self.nc = nc
        seq = self.seq
        L = DEPTH
        dr = {}

        def din(name, shape):
            dr[name] = nc.dram_tensor(name, list(shape), F32, kind="ExternalInput").ap()

        din("xin", (seq, D))
        din("meta", (NMETA, D))
        for f in ("ffn1", "ffn2"):
            din(f + "_w1", (L, D, DFF))
            din(f + "_w3", (L, D, DFF))
            din(f + "_w2", (L, DFF, D))
        din("w_in", (L, D, NIN))
        din("w_o_attn", (L, 512, D))
        din("w_o_conv", (L, 512, D))
        din("w_o_mlstm", (L, 512, D))
        din("w_out", (L, D, D))
        din("pv", (L, 128, NV))
        din("c32", (128, 1408))
        din("cbf", (128, 1408))
        dr["out"] = nc.dram_tensor("out", [seq, D], F32, kind="ExternalOutput").ap()
        self.dr = dr

        with ExitStack() as es:
            self.es = es

            def sb(name, shape, dt):
                return es.enter_context(nc.sbuf_tensor("sb_" + name, list(shape), dt))

            T = self
            T.c32 = sb("c32", [128, 1408], F32)
            T.cbf = sb("cbf", [128, 1408], BF16)
            T.pvt = sb("pvt", [128, L, NV], F32)
            T.ngb = sb("ngb", [128, L, 24], F32)
            T.sinkx = sb("sinkx", [128, L, 512], F32)
            T.sinke = sb("sinke", [128, L, 4], F32)
            T.xT = sb("xT", [128, KC, 512], F32)
            T.hT = sb("hT", [128, KC, 512], BF16)
            T.xs = sb("xs", [128, 2, D], F32)
            NSLOT = 5
            T.ring = sb("ring", [128, NSLOT, 4096], BF16)
            T.m = sb("m", [128, 11, 512], BF16)
            T.scf = sb("scf", [128, 6, 512], F32)
            T.scb = sb("scb", [128, 6, 520], BF16)
            T.qn = sb("qn", [128, 4, 512], BF16)
            T.kT = sb("kT", [128, L, 128 + 512], BF16)
            T.vtk = sb("vtk", [128, L, 5, 128], BF16)
            T.oa = sb("oa", [128, 4, 512], BF16)
            T.u = sb("u", [128, L, 4, 30 + 512], F32)
            T.acc = sb("acc", [128, 4, 512], F32)
            T.cb = sb("cb", [128, 4, 512], BF16)
            T.raw = sb("raw", [128, 2, 3 + 512], F32)
            T.qkh = sb("qkh", [128, L, 8, 3], F32)
            T.cacc = sb("cacc", [128, 2, 512], F32)
            T.mqk = sb("mqk", [128, 8, 512], BF16)
            T.ktok = sb("ktok", [128, 4, 4, 128], BF16)
            T.vaug = sb("vaug", [128, 4, 4, 130], BF16)
            T.so = sb("so", [128, 4, 512], BF16)
            T.hm = sb("hm", [128, 4, 512], BF16)
            T.Cst = sb("Cst", [128, L, 4, 129], F32)
            T.Cbf = sb("Cbf", [128, L, 4, 128], BF16)
            T.nbc = sb("nbc", [128, L, 4, 128], BF16)
            T.sm = sb("sm", [128, 8, 8], F32)
            T.mg = sb("mg", [128, 8, 512], BF16)
            ps = [es.enter_context(nc.psum_tensor("ps%d" % i, [128, 512], F32)) for i in range(8)]

            R = {}
            for nm, n in (("c", 1), ("pv", 1), ("xT", KC), ("hT", KC), ("xs", 2), ("m", 11), ("qn", 4), ("kT", L),
                          ("vtk", L), ("oa", 4), ("u", L * 4), ("acc", 4), ("cb", 4), ("raw", 2), ("qkh", L), ("cacc", 2),
                          ("mqk", 8), ("ktok", 4), ("vaug", 4), ("so", 4), ("hm", 4), ("Cst", L), ("Cbf", L), ("nbc", L),
                          ("sm", 2), ("mg", 8)):
                R[nm] = mkres(n)
            T.R = R
            T.pspool = Pool([(ps[i], Res()) for i in range(8)])
            T.fpool = Pool([(i, Res()) for i in range(6)])
            T.bpool = Pool([(i, Res()) for i in range(6)])
            T.slots = [(i, Res()) for i in range(NSLOT)]
            T.slot_next = 0

            self.setup()
            for ti, (pos, nb) in enumerate(self.tiles):
                self.load_tile(ti, pos, nb)
                self.stopped = (STOP == "load")
                for l in range(self.depth):
                    if not self.stopped:
                        self.ffn(l, 1, nb)
                        self.stopped = (STOP == "ffn1")
                    if not self.stopped:
                        self.mixer(l, nb, first=(ti == 0))
                    if not self.stopped:
                        self.ffn(l, 2, nb)
                self.store_tile(ti, pos, nb)
            self.P.emit(nc, es, [("out", 0), ("out", 1)])
        return nc

    def piece(self, dmas):
        i, res = self.slots[self.slot_next % len(self.slots)]
        self.slot_next += 1
        base = self.ring[:, i, :]
        self.slot_dmas(i, res, [(dst_fn(base), src) for dst_fn, src in dmas])
        return base, res

    def setup(self):
        T = self
        dr = T.dr
        R = T.R
        c = R["c"][0]
        self.dma("sp", T.c32[:], dr["c32"], "c32", [], [c])
        self.dma("pool", T.cbf[:], dr["cbf"], "cbf", [], [], lw_only=[c])
        self.dma("sp", T.pvt[:], dr["pv"].rearrange("l p n -> p l n"), "pv", [], [R["pv"][0]])
        T.ident_f = T.c32[:, 0:128]
        T.tri_f = T.c32[:, 128:256]
        T.maskb4 = T.c32[:, 256:768]
        T.ones_f = T.c32[:, 768:896]
        T.ident4_f = T.c32[:, 896:1408]
        T.ones_b = T.cbf[:, 0:128]
        T.blk_b = T.cbf[:, 128:256]
        T.ident_b = T.cbf[:, 256:384]
        T.mown4 = T.cbf[:, 384:896]
        T.mprev4 = T.cbf[:, 896:1408]
        pvr = R["pv"][0]
        for l in range(DEPTH):
            self.ts(T.ngb[:, l, :], T.pvt[:, l, PV_GB:PV_GB + 24], -1.0, None, ALU.mult, None, [pvr], [pvr])
            self.act(T.sinke[:, l, :], T.pvt[:, l, PV_SK:PV_SK + 4], AF.Exp, [pvr], [pvr])
            for j in range(4):
                self.ts(T.sinkx[:, l, j * 128:(j + 1) * 128], T.ones_f, T.sinke[:, l, j:j + 1], None, ALU.mult, None,
                        [pvr, c], [pvr])
            self.memset(T.kT[:, l, 0:128], 0.0, [R["kT"][l]])
            self.memset(T.vtk[:, l, 0, :], 0.0, [R["vtk"][l]])
            for cc in range(4):
                self.memset(T.u[:, l, cc, 0:30], 0.0, [R["u"][l * 4 + cc]])
            self.memset(T.qkh[:, l, :, :], 0.0, [R["qkh"][l]])
            self.memset(T.Cst[:, l, :, :], 0.0, [R["Cst"][l]])
            self.memset(T.Cbf[:, l, :, :], 0.0, [R["Cbf"][l]])
            self.memset(T.nbc[:, l, :, :], 0.0, [R["nbc"][l]])
        for b in range(4):
            self.memset(T.vaug[:, b, :, 128:129], 1.0, [R["vaug"][b]])

    def load_tile(self, ti, pos, nb):
        T = self
        dr = T.dr
        R = T.R
        nt = nb * 128
        tot = NMETA + T.seq
        for b in range(nb):
            p0 = pos + b * 128
            sl = b % 2
            xr = R["xs"][sl]
            nvalid = max(0, min(128, tot - p0))
            if nvalid < 128:
                self.memset(T.xs[:, sl, :], 0.0, [xr])
            if p0 == 0:
                self.dma("sp", T.xs[0:NMETA, sl, :], dr["meta"], ("xs", sl), [], [xr])
                self.dma("sp", T.xs[NMETA:128, sl, :], dr["xin"][0:128 - NMETA, :], ("xs", sl), [], [], lw_only=[xr])
            elif nvalid > 0:
                r0 = p0 - NMETA
                self.dma("sp", T.xs[0:nvalid, sl, :], dr["xin"][r0:r0 + nvalid, :], ("xs", sl), [], [xr])
            for h in range(2):
                pst, psr = T.pspool.get()
                for q in range(4):
                    kc = h * 4 + q
                    self.tr(pst[:, q * 128:(q + 1) * 128], T.xs[:, sl, kc * 128:(kc + 1) * 128], T.ident_f,
                            [xr, R["c"][0]], [psr])
                dst = T.xT[:, h * 4:(h + 1) * 4, b * 128:(b + 1) * 128]
                src = pst[:, :].rearrange("p (q t) -> p q t", q=4)
                if h == 0:
                    self.P.add("act", lambda e, dst=dst, src=src: e.activation(out=dst, in_=src, func=AF.Copy),
                               [psr], [R["xT"][k] for k in range(h * 4, h * 4 + 4)])
                else:
                    self.cp(dst, src, [psr], [R["xT"][k] for k in range(h * 4, h * 4 + 4)])
                T.pspool.put((pst, psr))

    def store_tile(self, ti, pos, nb):
        T = self
        dr = T.dr
        R = T.R
        tot = NMETA + T.seq
        for b in range(nb):
            p0 = pos + b * 128
            lo = max(p0, NMETA)
            hi = min(p0 + 128, tot)
            if hi <= lo:
                continue
            sl = b % 2
            xr = R["xs"][sl]
            for h in range(2):
                pst, psr = T.pspool.get()
                for q in range(4):
                    kc = h * 4 + q
                    self.tr(pst[:, q * 128:(q + 1) * 128], T.xT[:, kc, b * 128:(b + 1) * 128], T.ident_f,
                            [R["xT"][kc], R["c"][0]], [psr])
                dst = T.xs[:, sl, h * 512:(h + 1) * 512]
                if h == 0:
                    self.P.add("act", lambda e, dst=dst, src=pst: e.activation(out=dst, in_=src[:, :], func=AF.Copy),
                               [psr], [xr])
                else:
                    self.cp(dst, pst[:, :], [psr], [xr])
                T.pspool.put((pst, psr))
            self.dma("sp", dr["out"][lo - NMETA:hi - NMETA, :], T.xs[lo - p0:hi - p0, sl, :], ("out", sl), [xr], [])

    def rmsnorm(self, l, gcol, nb):
        T = self
        R = T.R
        nt = nb * 128
        pst, psr = T.pspool.get()
        for kc in range(KC):
            bi, br = T.bpool.get()
            sq = T.scb[:, bi, 0:nt]
            self.act(sq, T.xT[:, kc, 0:nt], AF.Square, [R["xT"][kc]], [br])
            self.mm(pst[:, 0:nt], T.ones_b, sq, kc == 0, kc == KC - 1, [br, R["c"][0]], [psr])
            T.bpool.put((bi, br))
        fi, fr = T.fpool.get()
        rstd = T.scf[:, fi, 0:nt]
        self.rsqrt(rstd, pst[:, 0:nt], 1.0 / D, 1e-6, [psr], fr)
        T.pspool.put((pst, psr))
        for kc in range(KC):
            self.stt(T.hT[:, kc, 0:nt], T.xT[:, kc, 0:nt], T.pvt[:, l, gcol + kc:gcol + kc + 1], rstd, ALU.mult, ALU.mult,
                     [R["xT"][kc], fr, R["pv"][0]], [R["hT"][kc]])
        T.fpool.put((fi, fr))

    def ffn(self, l, which, nb):
        T = self
        dr = T.dr
        R = T.R
        nt = nb * 128
        pre = "ffn%d" % which
        w1 = dr[pre + "_w1"][l].rearrange("(k p) f -> p k f", p=128)
        w3 = dr[pre + "_w3"][l].rearrange("(k p) f -> p k f", p=128)
        w2 = dr[pre + "_w2"][l].rearrange("(c p) n -> p c n", p=128)
        self.rmsnorm(l, PV_F1G if which == 1 else PV_F2G, nb)
        hres = R["hT"]
        for half in range(2):
            for pi in range(6):
                nf = 2 if pi < 5 else 1
                f0 = (half * 11 + 2 * pi) * 128
                wdt = nf * 128

                def d1(base, wdt=wdt):
                    return base[:, 0:KC * wdt].rearrange("p (k f) -> p k f", k=KC)

                def d3(base, wdt=wdt):
                    return base[:, 2048:2048 + KC * wdt].rearrange("p (k f) -> p k f", k=KC)

                base, wres = self.piece([(d1, w1[:, :, f0:f0 + wdt]), (d3, w3[:, :, f0:f0 + wdt])])
                wa = d1(base)
                wb = d3(base)
                for j in range(nf):
                    fl = 2 * pi + j
                    pa, par = T.pspool.get()
                    pb, pbr = T.pspool.get()
                    for kc in range(KC):
                        self.mm(pa[:, 0:nt], wa[:, kc, j * 128:(j + 1) * 128], T.hT[:, kc, 0:nt], kc == 0, kc == KC - 1,
                                [wres, hres[kc]], [par])
                    for kc in range(KC):
                        self.mm(pb[:, 0:nt], wb[:, kc, j * 128:(j + 1) * 128], T.hT[:, kc, 0:nt], kc == 0, kc == KC - 1,
                                [wres, hres[kc]], [pbr])
                    fi, fr = T.fpool.get()
                    s = T.scf[:, fi, 0:nt]
                    self.sigmoid(s, pa[:, 0:nt], [par], fr)
                    self.tt(s, s, pa[:, 0:nt], ALU.mult, [fr, par], [fr])
                    self.tt(T.m[:, fl, 0:nt], s, pb[:, 0:nt], ALU.mult, [fr, pbr], [R["m"][fl]])
                    T.fpool.put((fi, fr))
                    T.pspool.put((pa, par))
                    T.pspool.put((pb, pbr))
            for pi in range(4):
                c0 = pi * 256

                def d2(base):
                    return base[:, 0:11 * 256].rearrange("p (c n) -> p c n", c=11)

                base, wres = self.piece([(d2, w2[:, half * 11:(half + 1) * 11, c0:c0 + 256])])
                ww = d2(base)
                for j in range(2):
                    dc = pi * 2 + j
                    py, pyr = T.pspool.get()
                    for fl in range(11):
                        self.mm(py[:, 0:nt], ww[:, fl, j * 128:(j + 1) * 128], T.m[:, fl, 0:nt], fl == 0, fl == 10,
                                [wres, R["m"][fl]], [pyr])
                    self.stt(T.xT[:, dc, 0:nt], py[:, 0:nt], 0.5, T.xT[:, dc, 0:nt], ALU.mult, ALU.add,
                             [pyr, R["xT"][dc]], [R["xT"][dc]])
                    T.pspool.put((py, pyr))

    def mixer(self, l, nb, first):
        T = self
        dr = T.dr
        R = T.R
        nt = nb * 128
        cres = R["c"][0]
        pvr = R["pv"][0]
        hres = R["hT"]
        win = dr["w_in"][l].rearrange("(k p) n -> p k n", p=128)
        self.rmsnorm(l, PV_MXG, nb)

        def std_piece(c0, wdt):
            def dd(base, wdt=wdt):
                return base[:, 0:KC * wdt].rearrange("p (k n) -> p k n", k=KC)
            base, wres = self.piece([(dd, win[:, :, c0:c0 + wdt])])
            return dd(base), wres

        def proj_fm(wv, wres, col0, out_ps, out_res):
            for kc in range(KC):
                self.mm(out_ps[:, 0:nt], wv[:, kc, col0:col0 + 128], T.hT[:, kc, 0:nt], kc == 0, kc == KC - 1,
                        [wres, hres[kc]], [out_res])

        def dq(base):
            return base[:, 0:4096].rearrange("p (k c g d) -> p k g c d", k=KC, c=4, g=2, d=64)
        srcq = win[:, :, 0:512].rearrange("p k (g c d) -> p k g c d", g=2, c=4, d=64)
        i, qwres = T.slots[T.slot_next % len(T.slots)]
        T.slot_next += 1
        qbase = T.ring[:, i, :]
        self.slot_dmas(i, qwres, [(dq(qbase)[:, :, g, c, :], srcq[:, :, g, c, :]) for g in range(2) for c in range(4)])
        wq = qbase[:, 0:4096].rearrange("p (k n) -> p k n", k=KC)
        wkv, kvres = std_piece(512, 256)

        def qknorm(src_ps, src_res, gcol, lnmul, dst, dst_res):
            bi, br = T.bpool.get()
            sq = T.scb[:, bi, 0:nt]
            self.act(sq, src_ps[:, 0:nt], AF.Square, [src_res], [br])
            p2, p2r = T.pspool.get()
            self.mm(p2[:, 0:nt], T.blk_b, sq, True, True, [br, cres], [p2r])
            T.bpool.put((bi, br))
            fi, fr = T.fpool.get()
            r = T.scf[:, fi, 0:nt]
            self.rsqrt(r, p2[:, 0:nt], 1.0, 64 * 1e-6, [p2r], fr, lnmul=lnmul)
            T.pspool.put((p2, p2r))
            self.stt(dst, src_ps[:, 0:nt], T.pvt[:, l, gcol:gcol + 1], r, ALU.mult, ALU.mult, [src_res, fr, pvr], [dst_res])
            T.fpool.put((fi, fr))

        for c in range(4):
            pq, pqr = T.pspool.get()
            proj_fm(wq, qwres, c * 128, pq, pqr)
            qknorm(pq, pqr, PV_GQ, 0.0, T.qn[:, c, 0:nt], R["qn"][c])
            T.pspool.put((pq, pqr))
        pk, pkr = T.pspool.get()
        proj_fm(wkv, kvres, 0, pk, pkr)
        qknorm(pk, pkr, PV_GK, math.log(8.0), T.kT[:, l, 128:128 + nt], R["kT"][l])
        T.pspool.put((pk, pkr))
        for b in range(nb):
            pv_, pvr_ = T.pspool.get()
            for kc in range(KC):
                self.mm(pv_[:, 0:128], T.hT[:, kc, b * 128:(b + 1) * 128], wkv[:, kc, 128:256], kc == 0, kc == KC - 1,
                        [kvres, hres[kc]], [pvr_])
            self.cp(T.vtk[:, l, 1 + b, :], pv_[:, 0:128], [pvr_], [R["vtk"][l]])
            T.pspool.put((pv_, pvr_))
        ktr = R["kT"][l]
        vtr = R["vtk"][l]
        for qb in range(nb):
            has_prev = not (first and qb == 0)
            for g in range(2):
                gs_ = slice(g * 64, (g + 1) * 64)
                rhs_q = T.qn[gs_, :, qb * 128:(qb + 1) * 128]
                blocks = [(1, T.mown4)]
                if has_prev:
                    blocks.append((0, T.mprev4))
                ptiles = []
                for (own, mask) in blocks:
                    kcol = (128 + qb * 128) if own else (qb * 128)
                    pss, pssr = T.pspool.get()
                    self.mm(pss[:, :], T.kT[gs_, l, kcol:kcol + 128], rhs_q, True, True, [ktr] + R["qn"], [pssr])
                    bi, br = T.bpool.get()
                    pt = T.scb[:, bi, 0:512]
                    self.act(pt, pss[:, :], AF.Exp, [pssr], [br])
                    T.pspool.put((pss, pssr))
                    self.tt(pt, pt, mask, ALU.mult, [br, cres], [br])
                    ptiles.append((own, pt, bi, br))
                po, por = T.pspool.get()
                pd, pdr = T.pspool.get()
                n = len(ptiles)
                for ii, (own, pt, bi, br) in enumerate(ptiles):
                    vb = (1 + qb) if own else qb
                    self.mm(po[:, :], T.vtk[:, l, vb, :], pt, ii == 0, ii == n - 1, [vtr, br], [por])
                for ii, (own, pt, bi, br) in enumerate(ptiles):
                    self.mm(pd[:, :], T.ones_b, pt, ii == 0, ii == n - 1, [cres, br], [pdr])
                for (own, pt, bi, br) in ptiles:
                    T.bpool.put((bi, br))
                fi, fr = T.fpool.get()
                dt_ = T.scf[gs_, fi, 0:512]
                self.tt(dt_, pd[gs_, :], T.sinkx[gs_, l, :], ALU.add, [pdr, pvr], [fr])
                self.recip(dt_, dt_, [fr], [fr])
                T.pspool.put((pd, pdr))
                self.tt(T.oa[gs_, :, qb * 128:(qb + 1) * 128], po[gs_, :].rearrange("p (j q) -> p j q", j=4),
                        dt_.rearrange("p (j q) -> p j q", j=4), ALU.mult, [por, fr], R["oa"])
                T.fpool.put((fi, fr))
                T.pspool.put((po, por))
        self.act(T.kT[:, l, 0:128], T.kT[:, l, nt:nt + 128], AF.Copy, [ktr], [ktr])
        self.act(T.vtk[:, l, 0, :], T.vtk[:, l, nb, :], AF.Copy, [vtr], [vtr])

        if STOP == "attn":
            self.stopped = True
            return
        for pi in range(2):
            def da(base):
                return base[:, 0:2048].rearrange("p (k n) -> p k n", k=KC)

            def dg(base):
                return base[:, 2048:4096].rearrange("p (k n) -> p k n", k=KC)
            base, wres = self.piece([(da, win[:, :, 768 + pi * 256:768 + pi * 256 + 256]),
                                     (dg, win[:, :, 1280 + pi * 256:1280 + pi * 256 + 256])])
            wa, wg = da(base), dg(base)
            for j in range(2):
                c = pi * 2 + j
                pa, par = T.pspool.get()
                pg, pgr = T.pspool.get()
                proj_fm(wa, wres, j * 128, pa, par)
                proj_fm(wg, wres, j * 128, pg, pgr)
                fi, fr = T.fpool.get()
                s = T.scf[:, fi, 0:nt]
                self.sigmoid(s, pg[:, 0:nt], [pgr], fr)
                T.pspool.put((pg, pgr))
                self.tt(T.u[:, l, c, 30:30 + nt], s, pa[:, 0:nt], ALU.mult, [fr, par], [R["u"][l * 4 + c]])
                T.fpool.put((fi, fr))
                T.pspool.put((pa, par))
        def conv_tap(j, c):
            ur = R["u"][l * 4 + c]
            ar = R["acc"][c]
            wc = PV_CW + c * 31 + j
            eng = "pool" if c in POOL_CHUNKS else "dve"
            if j == 0:
                self.ts(T.acc[:, c, 0:nt], T.u[:, l, c, 0:nt], T.pvt[:, l, wc:wc + 1], T.pvt[:, l, PV_CB + c:PV_CB + c + 1],
                        ALU.mult, ALU.add, [ur, pvr], [ar], eng=eng)
            else:
                self.stt(T.acc[:, c, 0:nt], T.u[:, l, c, j:j + nt], T.pvt[:, l, wc:wc + 1], T.acc[:, c, 0:nt],
                         ALU.mult, ALU.add, [ur, pvr, ar], [ar], eng=eng)
        for j in range(31):
            for c in range(4):
                self.defer(lambda j=j, c=c: conv_tap(j, c))

        def conv_hist(c):
            ur = R["u"][l * 4 + c]
            self.act(T.u[:, l, c, 0:30], T.u[:, l, c, nt:nt + 30], AF.Copy, [ur], [ur])
        for c in range(4):
            self.defer(lambda c=c: conv_hist(c))

        def ln_all():
            pm, pmr = T.pspool.get()
            for c in range(4):
                bi, br = T.bpool.get()
                ab = T.scb[:, bi, 0:nt]
                self.act(ab, T.acc[:, c, 0:nt], AF.Copy, [R["acc"][c]], [br])
                self.mm(pm[:, 0:nt], T.ones_b, ab, c == 0, c == 3, [br, cres], [pmr])
                T.bpool.put((bi, br))
            for c in range(4):
                self.stt(T.acc[:, c, 0:nt], pm[:, 0:nt], -1.0 / 512, T.acc[:, c, 0:nt], ALU.mult, ALU.add,
                         [pmr, R["acc"][c]], [R["acc"][c]])
            T.pspool.put((pm, pmr))
            pv2, pv2r = T.pspool.get()
            for c in range(4):
                bi, br = T.bpool.get()
                sq = T.scb[:, bi, 0:nt]
                self.act(sq, T.acc[:, c, 0:nt], AF.Square, [R["acc"][c]], [br])
                self.mm(pv2[:, 0:nt], T.ones_b, sq, c == 0, c == 3, [br, cres], [pv2r])
                T.bpool.put((bi, br))
            fi, fr = T.fpool.get()
            rstd = T.scf[:, fi, 0:nt]
            self.rsqrt(rstd, pv2[:, 0:nt], 1.0 / 512, 1e-5, [pv2r], fr)
            T.pspool.put((pv2, pv2r))
            for c in range(4):
                ar = R["acc"][c]
                self.tt(T.acc[:, c, 0:nt], T.acc[:, c, 0:nt], rstd, ALU.mult, [ar, fr], [ar])
                self.act(T.acc[:, c, 0:nt], T.acc[:, c, 0:nt], AF.Identity, [ar, pvr], [ar],
                         scale=T.pvt[:, l, PV_LG + c:PV_LG + c + 1], bias=T.pvt[:, l, PV_LB + c:PV_LB + c + 1])
                f2, f2r = T.fpool.get()
                s_ = T.scf[:, f2, 0:nt]
                self.sigmoid(s_, T.acc[:, c, 0:nt], [ar], f2r)
                self.tt(T.cb[:, c, 0:nt], s_, T.acc[:, c, 0:nt], ALU.mult, [f2r, ar], [R["cb"][c]])
                T.fpool.put((f2, f2r))
            T.fpool.put((fi, fr))
        self.defer(ln_all)
        if STOP == "conv":
            self.pump()

        if STOP == "conv":
            self.stopped = True
            return
        hist = R["qkh"][l]
        for qk in range(2):
            wv, wres = std_piece(1792 + qk * 512, 512)
            for c in range(4):
                ch = qk * 4 + c
                pq, pqr = T.pspool.get()
                proj_fm(wv, wres, c * 128, pq, pqr)
                rs = ch % 2
                rr = R["raw"][rs]
                self.P.add("act", lambda e, o=T.raw[:, rs, 3:3 + nt], i_=pq[:, 0:nt]: e.activation(out=o, in_=i_, func=AF.Copy),
                           [pqr], [rr])
                T.pspool.put((pq, pqr))
                self.act(T.raw[:, rs, 0:3], T.qkh[:, l, ch, :], AF.Copy, [hist], [rr])
                car = R["cacc"][rs]
                wc = PV_QW + ch * 4
                self.ts(T.cacc[:, rs, 0:nt], T.raw[:, rs, 0:nt], T.pvt[:, l, wc:wc + 1], T.pvt[:, l, PV_QB + ch:PV_QB + ch + 1],
                        ALU.mult, ALU.add, [rr, pvr], [car])
                for j in range(1, 4):
                    self.stt(T.cacc[:, rs, 0:nt], T.raw[:, rs, j:j + nt], T.pvt[:, l, wc + j:wc + j + 1], T.cacc[:, rs, 0:nt],
                             ALU.mult, ALU.add, [rr, pvr, car], [car])
                self.act(T.qkh[:, l, ch, :], T.raw[:, rs, nt:nt + 3], AF.Copy, [rr], [hist])
                fi, fr = T.fpool.get()
                s = T.scf[:, fi, 0:nt]
                self.sigmoid(s, T.cacc[:, rs, 0:nt], [car], fr)
                self.tt(T.mqk[:, ch, 0:nt], s, T.cacc[:, rs, 0:nt], ALU.mult, [fr, car], [R["mqk"][ch]])
                T.fpool.put((fi, fr))
                self.pump(7)
        for b in range(nb):
            pst, psr = T.pspool.get()
            pstb = pst.bitcast(BF16)
            for h in range(4):
                self.tr(pstb[:, h * 128:(h + 1) * 128], T.mqk[:, 4 + h, b * 128:(b + 1) * 128], T.ident_b,
                        [R["mqk"][4 + h], cres], [psr])
            self.cp(T.ktok[:, b, :, :], pstb[:, 0:512].rearrange("p (h d) -> p h d", h=4), [psr], [R["ktok"][b]])
            T.pspool.put((pst, psr))
            self.pump(2)
        wv, wres = std_piece(2816, 512)
        for b in range(nb):
            pv_, pvr_ = T.pspool.get()
            for kc in range(KC):
                self.mm(pv_[:, :], T.hT[:, kc, b * 128:(b + 1) * 128], wv[:, kc, 0:512], kc == 0, kc == KC - 1,
                        [wres, hres[kc]], [pvr_])
            self.P.add("act", lambda e, o=T.vaug[:, b, :, 0:128], i_=pv_[:, :].rearrange("p (h d) -> p h d", h=4):
                       e.activation(out=o, in_=i_, func=AF.Copy), [pvr_], [R["vaug"][b]])
            T.pspool.put((pv_, pvr_))
        wv, wres = std_piece(3328, 512)
        for c in range(4):
            po, por = T.pspool.get()
            proj_fm(wv, wres, c * 128, po, por)
            fi, fr = T.fpool.get()
            s = T.scf[:, fi, 0:nt]
            self.sigmoid(s, po[:, 0:nt], [por], fr)
            T.pspool.put((po, por))
            self.cp(T.so[:, c, 0:nt], s, [fr], [R["so"][c]])
            T.fpool.put((fi, fr))
            self.pump(4)
        if STOP == "m3":
            self.stopped = True
            return
        def dgt(base):
            return base[:, 0:1024].rearrange("p (k n) -> p k n", k=KC)
        gbase, gwres = self.piece([(dgt, win[:, :, 3840:3968])])
        wgt = dgt(gbase)
        Cr, Cbr, nbr = R["Cst"][l], R["Cbf"][l], R["nbc"][l]
        isq = 1.0 / math.sqrt(128.0)
        for b in range(nb):
            cs = slice(b * 128, (b + 1) * 128)
            if b == nb - 1:
                self.pump()
            smi = b % 2
            smr = R["sm"][smi]
            sm = T.sm[:, smi * 4:(smi + 1) * 4, :]
            pg, pgr = T.pspool.get()
            for kc in range(KC):
                self.mm(pg[:, 0:16], T.hT[:, kc, cs], wgt[:, kc, 0:16], kc == 0, kc == KC - 1, [gwres, hres[kc]], [pgr])
            self.tt(sm[:, 0, :], pg[:, 0:8], T.pvt[:, l, PV_GT:PV_GT + 8], ALU.add, [pgr, pvr], [smr])
            T.pspool.put((pg, pgr))
            if STOP == "g1":
                continue
            self.act(sm[:, 1, 0:4], sm[:, 0, 4:8], AF.Exp, [smr], [smr], scale=-1.0)
            self.act(sm[:, 1, 0:4], sm[:, 1, 0:4], AF.Ln, [smr], [smr], bias=1.0)
            if STOP == "g2":
                continue
            fi, fr = T.fpool.get()
            nlb = T.scf[:, fi, 0:512]
            for h in range(4):
                self.ts(nlb[:, h * 128:(h + 1) * 128], T.ones_f, sm[:, 1, h:h + 1], None, ALU.mult, None, [cres, smr], [fr])
            pnb, pnbr = T.pspool.get()
            for h in range(4):
                self.mm(pnb[:, h * 128:(h + 1) * 128], nlb[:, h * 128:(h + 1) * 128], T.tri_f, True, True, [fr, cres], [pnbr])
            self.tt(nlb, pnb[:, :], T.ident4_f, ALU.mult, [pnbr, cres], [fr])
            self.P.add("dve", lambda e, o=sm[:, 1, 4:8], i_=nlb.rearrange("p (h t) -> p h t", h=4):
                       e.reduce_sum(out=o, in_=i_, axis=mybir.AxisListType.X), [fr], [smr])
            self.tt(sm[:, 1, 4:8], sm[:, 1, 4:8], sm[:, 0, 0:4], ALU.add, [smr], [smr])
            T.fpool.put((fi, fr))
            if STOP == "g3":
                T.pspool.put((pnb, pnbr))
                continue
            fd, fdr = T.fpool.get()
            Dm = T.scf[:, fd, 0:512]
            self.tt(Dm, T.maskb4, pnb[:, :], ALU.subtract, [cres, pnbr], [fdr])
            for h in range(4):
                self.act(Dm[:, h * 128:(h + 1) * 128], Dm[:, h * 128:(h + 1) * 128], AF.Exp, [fdr, smr], [fdr],
                         bias=sm[:, 1, 4 + h:5 + h])
            if STOP == "g4a":
                T.pspool.put((pnb, pnbr)); T.fpool.put((fd, fdr))
                continue
            fe, fer = T.fpool.get()
            eb = T.scf[:, fe, 0:512]
            self.act(eb, pnb[:, :], AF.Exp, [pnbr], [fer], scale=-1.0)
            if STOP == "g4b":
                T.pspool.put((pnb, pnbr)); T.fpool.put((fd, fdr)); T.fpool.put((fe, fer))
                continue
            T.pspool.put((pnb, pnbr))
            self.act(sm[:, 2, 0:4], sm[:, 1, 4:8], AF.Exp, [smr], [smr])
            ebv = eb.rearrange("p (h t) -> p h t", h=4)
            self.tt(sm[:, 3, 0:4], sm[:, 2, 0:4], ebv[:, :, 127], ALU.mult, [smr, fer], [smr])
            if STOP == "m4":
                T.fpool.put((fe, fer)); T.fpool.put((fd, fdr))
                continue
            bq, bqr = T.bpool.get()
            qs = T.scb[:, bq, 0:512]
            self.stt(qs.rearrange("p (h t) -> p h t", h=4), T.mqk[:, 0:4, cs], isq, eb.rearrange("p (h t) -> p h t", h=4),
                     ALU.mult, ALU.mult, R["mqk"][0:4] + [fer], [bqr])
            self.pump(8)
            pgm, pgmr = T.pspool.get()
            for h in range(4):
                self.mm(pgm[:, h * 128:(h + 1) * 128], T.mqk[:, 4 + h, cs], T.mqk[:, h, cs], True, True,
                        [R["mqk"][4 + h], R["mqk"][h]], [pgmr])
            bp, bpr = T.bpool.get()
            Pm = T.scb[:, bp, 0:512]
            self.stt(Pm, pgm[:, :], isq, Dm, ALU.mult, ALU.mult, [pgmr, fdr], [bpr])
            T.pspool.put((pgm, pgmr))
            T.fpool.put((fd, fdr))
            pn, pnr = T.pspool.get()
            pd, pdr = T.pspool.get()
            for h in range(4):
                hs = slice(h * 128, (h + 1) * 128)
                self.mm(pn[:, hs], T.vaug[:, b, h, 0:128], Pm[:, hs], True, False, [R["vaug"][b], bpr], [pnr])
                self.mm(pn[:, hs], T.Cbf[:, l, h, :], qs[:, hs], False, True, [Cbr, bqr], [pnr])
            for h in range(4):
                hs = slice(h * 128, (h + 1) * 128)
                self.mm(pd[:, hs], T.ones_b, Pm[:, hs], True, False, [cres, bpr], [pdr])
                self.mm(pd[:, hs], T.nbc[:, l, h, :], qs[:, hs], False, True, [nbr, bqr], [pdr])
            T.bpool.put((bp, bpr))
            T.bpool.put((bq, bqr))
            fn_, fnr = T.fpool.get()
            dn = T.scf[:, fn_, 0:512]
            self.act(dn, pd[:, :], AF.Abs, [pdr], [fnr])
            T.pspool.put((pd, pdr))
            self.ts(dn, dn, 1.0, None, ALU.max, None, [fnr], [fnr])
            self.recip(dn, dn, [fnr], [fnr])
            self.tt(dn, pn[:, :], dn, ALU.mult, [pnr, fnr], [fnr])
            T.pspool.put((pn, pnr))
            self.tt(T.hm[:, :, cs], dn.rearrange("p (h t) -> p h t", h=4), T.so[:, :, cs], ALU.mult, [fnr] + R["so"], R["hm"])
            T.fpool.put((fn_, fnr))
            self.pump(8)
            bv, bvr = T.bpool.get()
            vw = T.scb[:, bv, 0:520].rearrange("p (h e) -> p h e", h=4)
            for h in range(4):
                self.ts(vw[:, h, 0:129], T.vaug[:, b, h, 0:129], sm[:, 3, h:h + 1], None, ALU.mult, None, [R["vaug"][b], smr], [bvr])
            pc0, pc0r = T.pspool.get()
            pc1, pc1r = T.pspool.get()
            for h in range(4):
                pcc, pccr = (pc0, pc0r) if h < 2 else (pc1, pc1r)
                o = (h % 2) * 130
                self.mm(pcc[:, o:o + 129], T.ktok[:, b, h, :], vw[:, h, 0:129], True, True, [R["ktok"][b], bvr], [pccr])
            T.bpool.put((bv, bvr))
            for h in range(4):
                pcc, pccr = (pc0, pc0r) if h < 2 else (pc1, pc1r)
                o = (h % 2) * 130
                self.stt(T.Cst[:, l, h, :], T.Cst[:, l, h, :], eb[:, h * 128 + 127:h * 128 + 128], pcc[:, o:o + 129],
                         ALU.mult, ALU.add, [Cr, fer, pccr], [Cr])
            T.pspool.put((pc0, pc0r))
            T.pspool.put((pc1, pc1r))
            T.fpool.put((fe, fer))
            self.P.add("act", lambda e, o=T.Cbf[:, l, :, :], i_=T.Cst[:, l, :, 0:128]: e.activation(out=o, in_=i_, func=AF.Copy),
                       [Cr], [Cbr])
            for h in range(4):
                self.act(T.nbc[:, l, h, :], T.ones_f, AF.Identity, [cres, Cr], [nbr], scale=T.Cst[:, l, h, 128:129])

        self.pump()
        if STOP in ("mlstm", "m4", "g1", "g2", "g3", "g4a", "g4b"):
            self.stopped = True
            return
        woa = dr["w_o_attn"][l]
        woc = dr["w_o_conv"][l].rearrange("(j p) n -> p j n", p=128)
        wom = dr["w_o_mlstm"][l].rearrange("(j p) n -> p j n", p=128)
        zg = win[:, :, 3848:6920].rearrange("p k (b n) -> p b k n", b=3)
        for dc in range(8):
            ds_ = slice(dc * 128, (dc + 1) * 128)

            def dz(base):
                return base[:, 0:3072].rearrange("p (b k n) -> p b k n", b=3, k=KC)
            iz, zres = T.slots[T.slot_next % len(T.slots)]
            T.slot_next += 1
            zbase = T.ring[:, iz, :]
            wz = dz(zbase)
            self.slot_dmas(iz, zres, [(wz[:, b_, :, :], zg[:, b_, :, ds_]) for b_ in range(3)])
            i2, ores = T.slots[T.slot_next % len(T.slots)]
            T.slot_next += 1
            obase = T.ring[:, i2, :]
            wo = obase[:, 0:1536].rearrange("p (b j n) -> p b j n", b=3, j=4)
            prs = []
            for g in range(2):
                src = woa[g * 256:(g + 1) * 256, ds_].rearrange("(j d) n -> d j n", d=64)
                prs.append((wo[g * 64:(g + 1) * 64, 0, :, :], src))
            prs.append((wo[:, 1, :, :], woc[:, :, ds_]))
            prs.append((wo[:, 2, :, :], wom[:, :, ds_]))
            self.slot_dmas(i2, ores, prs)
            gts = []
            for br_ in range(3):
                pz, pzr = T.pspool.get()
                for kc in range(KC):
                    self.mm(pz[:, 0:nt], wz[:, br_, kc, :], T.hT[:, kc, 0:nt], kc == 0, kc == KC - 1, [zres, hres[kc]], [pzr])
                fi, fr = T.fpool.get()
                s = T.scf[:, fi, 0:nt]
                self.sigmoid(s, pz[:, 0:nt], [pzr, pvr], fr, negbias=T.ngb[:, l, br_ * 8 + dc:br_ * 8 + dc + 1])
                T.pspool.put((pz, pzr))
                gts.append((s, fi, fr))
            srcs = [(T.oa, R["oa"]), (T.cb, R["cb"]), (T.hm, R["hm"])]
            for br_ in range(3):
                py, pyr = T.pspool.get()
                buf, bres = srcs[br_]
                for j in range(4):
                    self.mm(py[:, 0:nt], wo[:, br_, j, :], buf[:, j, 0:nt], j == 0, j == 3, [ores, bres[j]], [pyr])
                s, fi, fr = gts[br_]
                self.tt(s, s, py[:, 0:nt], ALU.mult, [fr, pyr], [fr])
                T.pspool.put((py, pyr))
            s0, f0, r0 = gts[0]
            s1, f1, r1 = gts[1]
            s2, f2, r2 = gts[2]
            self.tt(s0, s0, s1, ALU.add, [r0, r1], [r0])
            self.tt(T.mg[:, dc, 0:nt], s0, s2, ALU.add, [r0, r2], [R["mg"][dc]])
            for (s, fi, fr) in gts:
                T.fpool.put((fi, fr))
        wout = dr["w_out"][l].rearrange("(k p) n -> p k n", p=128)
        for pi in range(2):
            def dd(base):
                return base[:, 0:4096].rearrange("p (k n) -> p k n", k=KC)
            base, wres = self.piece([(dd, wout[:, :, pi * 512:(pi + 1) * 512])])
            wv = dd(base)
            for j in range(4):
                dc = pi * 4 + j
                py, pyr = T.pspool.get()
                for kc in range(KC):
                    self.mm(py[:, 0:nt], wv[:, kc, j * 128:(j + 1) * 128], T.mg[:, kc, 0:nt], kc == 0, kc == KC - 1,
                            [wres, R["mg"][kc]], [pyr])
                self.tt(T.xT[:, dc, 0:nt], py[:, 0:nt], T.xT[:, dc, 0:nt], ALU.add, [pyr, R["xT"][dc]], [R["xT"][dc]])
                T.pspool.put((py, pyr))


def make_consts():
    c32 = np.zeros((128, 1408), np.float32)
    r = np.arange(128)
    c32[:, 0:128] = np.eye(128, dtype=np.float32)
    tri = (r[:, None] <= r[None, :]).astype(np.float32)
    c32[:, 128:256] = tri
    maskb = np.where(r[:, None] <= r[None, :], 0.0, -30000.0).astype(np.float32)
    c32[:, 256:768] = np.tile(maskb, (1, 4))
    c32[:, 768:896] = 1.0
    c32[:, 896:1408] = np.tile(np.eye(128, dtype=np.float32), (1, 4))
    cbf = np.zeros((128, 1408), np.float32)
    cbf[:, 0:128] = 1.0
    blk = np.zeros((128, 128), np.float32)
    blk[0:64, 0:64] = 1.0
    blk[64:128, 64:128] = 1.0
    cbf[:, 128:256] = blk
    cbf[:, 256:384] = np.eye(128, dtype=np.float32)
    cbf[:, 384:896] = np.tile(tri, (1, 4))
    cbf[:, 896:1408] = np.tile(1.0 - tri, (1, 4))
    return c32, cbf


def make_pv(inp):
    L = DEPTH
    pv = np.zeros((L, 128, NV), np.float32)
    for l in range(L):
        def fm(v, n):
            return np.ascontiguousarray(np.asarray(v, np.float32).reshape(n, 128).T)
        pv[l, :, PV_F1G:PV_F1G + 8] = fm(inp["ffn1_norm"][l], 8)
        pv[l, :, PV_MXG:PV_MXG + 8] = fm(inp["mix_norm"][l], 8)
        pv[l, :, PV_F2G:PV_F2G + 8] = fm(inp["ffn2_norm"][l], 8)
        gb = np.asarray(inp["gate_bias"][l], np.float32)
        for b in range(3):
            pv[l, :, PV_GB + b * 8:PV_GB + b * 8 + 8] = fm(gb[b], 8)
        cw = np.asarray(inp["conv_dw_w"][l], np.float32)
        for c in range(4):
            pv[l, :, PV_CW + c * 31:PV_CW + (c + 1) * 31] = cw[:, c * 128:(c + 1) * 128].T
        pv[l, :, PV_CB:PV_CB + 4] = fm(inp["conv_dw_b"][l], 4)
        pv[l, :, PV_LG:PV_LG + 4] = fm(inp["conv_ln_g"][l], 4)
        pv[l, :, PV_LB:PV_LB + 4] = fm(inp["conv_ln_b"][l], 4)
        qw = np.asarray(inp["mlstm_qk_conv_w"][l], np.float32)
        for c in range(8):
            pv[l, :, PV_QW + c * 4:PV_QW + (c + 1) * 4] = qw[:, c * 128:(c + 1) * 128].T
        pv[l, :, PV_QB:PV_QB + 8] = fm(inp["mlstm_qk_conv_b"][l], 8)
        pv[l, :, PV_GQ] = np.tile(np.asarray(inp["attn_q_norm"][l], np.float32), 2)
        pv[l, :, PV_GK] = np.tile(np.asarray(inp["attn_k_norm"][l], np.float32), 2)
        sk = np.asarray(inp["attn_sinks"][l], np.float32)
        pv[l, 0:64, PV_SK:PV_SK + 4] = sk[None, 0:4]
        pv[l, 64:128, PV_SK:PV_SK + 4] = sk[None, 4:8]
        pv[l, :, PV_GT:PV_GT + 4] = np.asarray(inp["mlstm_igate_bias"][l], np.float32)[None, :]
        pv[l, :, PV_GT + 4:PV_GT + 8] = np.asarray(inp["mlstm_fgate_bias"][l], np.float32)[None, :]
    return pv


_NC_CACHE = {}


def run(inputs, n_cores=None, depth=DEPTH, trace=False):
    x = np.asarray(inputs["x"], np.float32)
    bsz, seq, _ = x.shape
    if n_cores is None:
        n_cores = bsz
    key = (seq, depth, STOP)
    if key not in _NC_CACHE:
        _NC_CACHE[key] = Builder(seq, depth).build()
    nc = _NC_CACHE[key]
    c32, cbf = make_consts()
    pv = make_pv(inputs)
    shared = {"meta": np.ascontiguousarray(np.asarray(inputs["meta_tokens"], np.float32)), "pv": pv, "c32": c32, "cbf": cbf}
    for k in ("ffn1_w1", "ffn1_w3", "ffn1_w2", "ffn2_w1", "ffn2_w3", "ffn2_w2", "w_in", "w_o_attn", "w_o_conv",
              "w_o_mlstm", "w_out"):
        shared[k] = np.ascontiguousarray(np.asarray(inputs[k], np.float32))
    in_maps = []
    for b in range(n_cores):
        d = dict(shared)
        d["xin"] = np.ascontiguousarray(x[b])
        in_maps.append(d)
    res = run_bass_kernel_spmd(nc, in_maps, core_ids=list(range(n_cores)), **({"trace": True} if trace else {}))
    out = np.stack([np.asarray(r["out"], np.float32) for r in res.results], axis=0)
    return out, res


def kernel(**inputs):
    out, _ = run(inputs)
    return out
```

```python
import math
from collections import deque
from contextlib import ExitStack

import numpy as np
import concourse.bass as bass
import concourse.mybir as mybir
from concourse.bass_utils import run_bass_kernel_spmd

F32 = mybir.dt.float32
BF16 = mybir.dt.bfloat16
AF = mybir.ActivationFunctionType
ALU = mybir.AluOpType

D = 1024
KC = 8
DFF = 2816
NIN = 6920
NMETA = 16
DEPTH = 2
ENGS = ("pe", "act", "dve", "pool", "sp")
STOP = None
POOL_CHUNKS = ()
NSW = 40

PV_F1G, PV_MXG, PV_F2G, PV_GB, PV_CW, PV_CB, PV_LG, PV_LB, PV_QW, PV_QB, PV_GQ, PV_GK, PV_SK, PV_GT, NV = (
    0, 8, 16, 24, 48, 172, 176, 180, 184, 216, 224, 225, 226, 230, 238)


class Res:
    __slots__ = ("lw", "rd")

    def __init__(self):
        self.lw = []
        self.rd = {}


def mkres(n):
    return [Res() for _ in range(n)]


class Prog:
    def __init__(self):
        self.st = {e: [] for e in ENGS}
        self.seen = {e: {} for e in ENGS}
        self.dmacnt = {}
        self.nsw = 0

    def add(self, eng, fn, reads=(), writes=(), dmakey=None, lw_only=()):
        st = self.st[eng]
        idx = len(st)
        need = {}
        isdma = dmakey is not None
        if dmakey is None:
            tok = ("e", eng, idx)
        else:
            if eng == "pool":
                dmakey = ("sw", self.nsw % NSW)
                self.nsw += 1
                prev = self.dmacnt.get(dmakey, 0)
                if prev:
                    need[("d", dmakey)] = prev
            c = self.dmacnt.get(dmakey, 0) + 1
            self.dmacnt[dmakey] = c
            tok = ("d", dmakey, c)

        def want(t, raw):
            if t is None:
                return
            kind, key, val = t
            if kind == "e" and key == eng and not isdma:
                if (not raw) or eng == "pe":
                    return
            k = (kind, key)
            if need.get(k, -1) < val:
                need[k] = val

        for r in reads:
            for t_ in r.lw:
                want(t_, True)
        for w in writes:
            for t_ in w.lw:
                want(t_, False)
            for k, v in w.rd.items():
                want((k[0], k[1], v), False)
        waits = []
        sn = self.seen[eng]
        for k, v in need.items():
            if sn.get(k, -1) < v:
                sn[k] = v
                waits.append((k[0], k[1], v))
        st.append((fn, waits, dmakey))
        k = (tok[0], tok[1])
        for r in reads:
            if r.rd.get(k, -1) < tok[2]:
                r.rd[k] = tok[2]
        for w in writes:
            w.lw = [tok]
            w.rd = {}
        for w in lw_only:
            w.lw.append(tok)
        return tok

    def emit(self, nc, es, final_dma_keys):
        ranks = {e: {} for e in ENGS}
        used = {e: set() for e in ENGS}
        for e in ENGS:
            for (_, waits, _) in self.st[e]:
                for kind, key, val in waits:
                    if kind == "e":
                        used[key].add(val)
        for e in ENGS:
            for i, idx in enumerate(sorted(used[e])):
                ranks[e][idx] = i + 1
        esem = {e: es.enter_context(nc.semaphore("sem_" + e)) for e in ENGS}
        dsem = {k: es.enter_context(nc.semaphore("dsem%d" % n)) for n, k in enumerate(self.dmacnt)}
        block = es.enter_context(nc.Block())
        st = self.st
        dmacnt = self.dmacnt

        def run(e, eng):
            rk = ranks[eng]
            for i, (fn, waits, dmakey) in enumerate(st[eng]):
                for kind, key, val in waits:
                    if kind == "e":
                        e.wait_ge(esem[key], ranks[key][val])
                    else:
                        e.wait_ge(dsem[key], 16 * val)
                ins = fn(e)
                if dmakey is not None:
                    ins.then_inc(dsem[dmakey], 16)
                elif i in rk:
                    ins.then_inc(esem[eng], 1)
            if eng == "sp":
                for k in final_dma_keys:
                    if k in dmacnt:
                        e.wait_ge(dsem[k], 16 * dmacnt[k])

        @block.tensor
        def _(e):
            run(e, "pe")

        @block.scalar
        def _(e):
            run(e, "act")

        @block.vector
        def _(e):
            run(e, "dve")

        @block.gpsimd
        def _(e):
            run(e, "pool")

        @block.sync
        def _(e):
            run(e, "sp")


class Pool:
    def __init__(self, items):
        self.free = deque(items)

    def get(self):
        assert self.free, "scratch pool exhausted"
        return self.free.popleft()

    def put(self, it):
        self.free.append(it)


class Builder:
    def __init__(self, seq, depth=DEPTH, nt_max=512):
        self.seq = seq
        self.depth = depth
        tot = NMETA + seq
        self.tiles = []
        pos = 0
        while pos < tot:
            nb = min(nt_max // 128, (tot - pos + 127) // 128)
            self.tiles.append((pos, nb))
            pos += nb * 128
        self.P = Prog()
        self.dq = deque()

    def defer(self, fn):
        self.dq.append(fn)

    def pump(self, n=None):
        k = 0
        while self.dq and (n is None or k < n):
            self.dq.popleft()()
            k += 1

    def mm(self, out, lhsT, rhs, start, stop, reads, writes):
        self.P.add("pe", lambda e: e.matmul(out, lhsT=lhsT, rhs=rhs, start=start, stop=stop), reads, writes)

    def tr(self, out, in_, ident, reads, writes):
        self.P.add("pe", lambda e: e.transpose(out, in_, ident), reads, writes)

    def act(self, out, in_, func, reads, writes, bias=None, scale=None):
        kw = {}
        if bias is not None:
            kw["bias"] = bias
        if scale is not None:
            kw["scale"] = scale
        self.P.add("act", lambda e: e.activation(out=out, in_=in_, func=func, **kw), reads, writes)

    def tt(self, out, in0, in1, op, reads, writes, eng="dve"):
        self.P.add(eng, lambda e: e.tensor_tensor(out=out, in0=in0, in1=in1, op=op), reads, writes)

    def ts(self, out, in0, s1, s2, op0, op1, reads, writes, eng="dve"):
        if s2 is None:
            self.P.add(eng, lambda e: e.tensor_scalar(out=out, in0=in0, scalar1=s1, scalar2=None, op0=op0), reads, writes)
        else:
            self.P.add(eng, lambda e: e.tensor_scalar(out=out, in0=in0, scalar1=s1, scalar2=s2, op0=op0, op1=op1), reads, writes)

    def stt(self, out, in0, scalar, in1, op0, op1, reads, writes, eng="dve"):
        self.P.add(eng, lambda e: e.scalar_tensor_tensor(out=out, in0=in0, scalar=scalar, in1=in1, op0=op0, op1=op1), reads, writes)

    def cp(self, out, in_, reads, writes, eng="dve"):
        self.P.add(eng, lambda e: e.tensor_copy(out=out, in_=in_), reads, writes)

    def recip(self, out, in_, reads, writes):
        self.P.add("dve", lambda e: e.reciprocal(out=out, in_=in_), reads, writes)

    def memset(self, ap, val, writes, eng="dve"):
        self.P.add(eng, lambda e: e.memset(ap, val), (), writes)

    def dma(self, eng, out, in_, key, reads, writes, lw_only=()):
        self.P.add(eng, lambda e: e.dma_start(out=out, in_=in_), reads, writes, dmakey=key, lw_only=lw_only)

    def slot_dmas(self, i, res, pairs):
        for n, (dst, src) in enumerate(pairs):
            if n == 0:
                self.dma("pool", dst, src, ("w", i), [], [res])
            else:
                self.dma("pool", dst, src, ("w", i), [], [], lw_only=[res])

    def sigmoid(self, out, in_, reads, wres, negbias=None):
        self.act(out, in_, AF.Exp, reads, [wres], scale=-1.0, bias=negbias)
        self.act(out, out, AF.Ln, [wres], [wres], bias=1.0)
        self.act(out, out, AF.Exp, [wres], [wres], scale=-1.0)

    def rsqrt(self, out, in_, scale, eps, reads, wres, lnmul=0.0):
        self.act(out, in_, AF.Ln, reads, [wres], scale=scale, bias=eps)
        if lnmul != 0.0:
            self.act(out, out, AF.Exp, [wres], [wres], scale=-0.5, bias=lnmul)
        else:
            self.act(out, out, AF.Exp, [wres], [wres], scale=-0.5)

    def build(self):
        nc = bass.Bass("TRN2", target_bir_lowering=False)
        self.nc = nc
        seq = self.seq
        L = DEPTH
        dr = {}

        def din(name, shape):
            dr[name] = nc.dram_tensor(name, list(shape), F32, kind="ExternalInput").ap()

        din("xin", (seq, D))
        din("meta", (NMETA, D))
        for f in ("ffn1", "ffn2"):
            din(f + "_w1", (L, D, DFF))
            din(f + "_w3", (L, D, DFF))
            din(f + "_w2", (L, DFF, D))
        din("w_in", (L, D, NIN))
        din("w_o_attn", (L, 512, D))
        din("w_o_conv", (L, 512, D))
        din("w_o_mlstm", (L, 512, D))
        din("w_out", (L, D, D))
        din("pv", (L, 128, NV))
        din("c32", (128, 1408))
        din("cbf", (128, 1408))
        dr["out"] = nc.dram_tensor("out", [seq, D], F32, kind="ExternalOutput").ap()
        self.dr = dr

        with ExitStack() as es:
            self.es = es

            def sb(name, shape, dt):
                return es.enter_context(nc.sbuf_tensor("sb_" + name, list(shape), dt))

            T = self
            T.c32 = sb("c32", [128, 1408], F32)
            T.cbf = sb("cbf", [128, 1408], BF16)
            T.pvt = sb("pvt", [128, L, NV], F32)
            T.ngb = sb("ngb", [128, L, 24], F32)
            T.sinkx = sb("sinkx", [128, L, 512], F32)
            T.sinke = sb("sinke", [128, L, 4], F32)
            T.xT = sb("xT", [128, KC, 512], F32)
            T.hT = sb("hT", [128, KC, 512], BF16)
            T.xs = sb("xs", [128, 2, D], F32)
            NSLOT = 5
            T.ring = sb("ring", [128, NSLOT, 4096], BF16)
            T.m = sb("m", [128, 11, 512], BF16)
            T.scf = sb("scf", [128, 6, 512], F32)
            T.scb = sb("scb", [128, 6, 520], BF16)
            T.qn = sb("qn", [128, 4, 512], BF16)
            T.kT = sb("kT", [128, L, 128 + 512], BF16)
            T.vtk = sb("vtk", [128, L, 5, 128], BF16)
            T.oa = sb("oa", [128, 4, 512], BF16)
            T.u = sb("u", [128, 4, 30 + 512], F32)
            T.uh = sb("uh", [128, L, 4, 30], F32)
            T.vw = sb("vw", [128, 4, 520], BF16)
            T.acc = sb("acc", [128, 4, 512], F32)
            T.cb = sb("cb", [128, 4, 512], BF16)
            T.raw = sb("raw", [128, 2, 3 + 512], F32)
            T.qkh = sb("qkh", [128, L, 8, 3], F32)
            T.cacc = sb("cacc", [128, 2, 512], F32)
            T.mqk = sb("mqk", [128, 8, 512], BF16)
            T.ktok = sb("ktok", [128, 4, 4, 128], BF16)
            T.vaug = sb("vaug", [128, 4, 4, 130], BF16)
            T.so = sb("so", [128, 4, 512], BF16)
            T.hm = sb("hm", [128, 4, 512], BF16)
            T.Cst = sb("Cst", [128, L, 4, 129], F32)
            T.Cbf = sb("Cbf", [128, L, 4, 128], BF16)
            T.nbc = sb("nbc", [128, L, 4, 128], BF16)
            T.sm = sb("sm", [128, 16, 8], F32)
            T.mg = sb("mg", [128, 8, 512], BF16)
            ps = [es.enter_context(nc.psum_tensor("ps%d" % i, [128, 512], F32)) for i in range(8)]

            R = {}
            for nm, n in (("c", 1), ("pv", 1), ("xT", KC), ("hT", KC), ("xs", 2), ("m", 11), ("qn", 4), ("kT", L),
                          ("vtk", L), ("oa", 4), ("u", 4), ("acc", 4), ("cb", 4), ("raw", 2), ("qkh", L), ("cacc", 2),
                          ("mqk", 8), ("ktok", 4), ("vaug", 4), ("so", 4), ("hm", 4), ("Cst", L), ("Cbf", L), ("nbc", L),
                          ("sm", 4), ("vw", 4), ("uh", L), ("mg", 8)):
                R[nm] = mkres(n)
            T.R = R
            T.pspool = Pool([(ps[i], Res()) for i in range(8)])
            T.fpool = Pool([(i, Res()) for i in range(6)])
            T.bpool = Pool([(i, Res()) for i in range(6)])
            T.slots = [(i, Res()) for i in range(NSLOT)]
            T.slot_next = 0

            self.setup()
            for ti, (pos, nb) in enumerate(self.tiles):
                self.load_tile(ti, pos, nb)
                self.stopped = (STOP == "load")
                for l in range(self.depth):
                    if not self.stopped:
                        self.ffn(l, 1, nb)
                        self.stopped = (STOP == "ffn1")
                    if not self.stopped:
                        self.mixer(l, nb, first=(ti == 0))
                    if not self.stopped:
                        self.ffn(l, 2, nb)
                self.store_tile(ti, pos, nb)
            self.P.emit(nc, es, [("out", 0), ("out", 1)])
        return nc

    def piece(self, dmas):
        i, res = self.slots[self.slot_next % len(self.slots)]
        self.slot_next += 1
        base = self.ring[:, i, :]
        self.slot_dmas(i, res, [(dst_fn(base), src) for dst_fn, src in dmas])
        return base, res

    def setup(self):
        T = self
        dr = T.dr
        R = T.R
        c = R["c"][0]
        self.dma("sp", T.c32[:], dr["c32"], "c32", [], [c])
        self.dma("pool", T.cbf[:], dr["cbf"], "cbf", [], [], lw_only=[c])
        self.dma("sp", T.pvt[:], dr["pv"].rearrange("l p n -> p l n"), "pv", [], [R["pv"][0]])
        T.ident_f = T.c32[:, 0:128]
        T.tri_f = T.c32[:, 128:256]
        T.maskb4 = T.c32[:, 256:768]
        T.ones_f = T.c32[:, 768:896]
        T.ident4_f = T.c32[:, 896:1408]
        T.ones_b = T.cbf[:, 0:128]
        T.blk_b = T.cbf[:, 128:256]
        T.ident_b = T.cbf[:, 256:384]
        T.mown4 = T.cbf[:, 384:896]
        T.mprev4 = T.cbf[:, 896:1408]
        pvr = R["pv"][0]
        for l in range(DEPTH):
            self.ts(T.ngb[:, l, :], T.pvt[:, l, PV_GB:PV_GB + 24], -1.0, None, ALU.mult, None, [pvr], [pvr])
            self.act(T.sinke[:, l, :], T.pvt[:, l, PV_SK:PV_SK + 4], AF.Exp, [pvr], [pvr])
            for j in range(4):
                self.ts(T.sinkx[:, l, j * 128:(j + 1) * 128], T.ones_f, T.sinke[:, l, j:j + 1], None, ALU.mult, None,
                        [pvr, c], [pvr])
            self.memset(T.kT[:, l, 0:128], 0.0, [R["kT"][l]])
            self.memset(T.vtk[:, l, 0, :], 0.0, [R["vtk"][l]])
            self.memset(T.uh[:, l, :, :], 0.0, [R["uh"][l]])
            self.memset(T.qkh[:, l, :, :], 0.0, [R["qkh"][l]])
            self.memset(T.Cst[:, l, :, :], 0.0, [R["Cst"][l]])
            self.memset(T.Cbf[:, l, :, :], 0.0, [R["Cbf"][l]])
            self.memset(T.nbc[:, l, :, :], 0.0, [R["nbc"][l]])
        for b in range(4):
            self.memset(T.vaug[:, b, :, 128:129], 1.0, [R["vaug"][b]])

    def load_tile(self, ti, pos, nb):
        T = self
        dr = T.dr
        R = T.R
        nt = nb * 128
        tot = NMETA + T.seq
        for b in range(nb):
            p0 = pos + b * 128
            sl = b % 2
            xr = R["xs"][sl]
            nvalid = max(0, min(128, tot - p0))
            if nvalid < 128:
                self.memset(T.xs[:, sl, :], 0.0, [xr])
            if p0 == 0:
                self.dma("sp", T.xs[0:NMETA, sl, :], dr["meta"], ("xs", sl), [], [xr])
                self.dma("sp", T.xs[NMETA:128, sl, :], dr["xin"][0:128 - NMETA, :], ("xs", sl), [], [], lw_only=[xr])
            elif nvalid > 0:
                r0 = p0 - NMETA
                self.dma("sp", T.xs[0:nvalid, sl, :], dr["xin"][r0:r0 + nvalid, :], ("xs", sl), [], [xr])
            for h in range(2):
                pst, psr = T.pspool.get()
                for q in range(4):
                    kc = h * 4 + q
                    self.tr(pst[:, q * 128:(q + 1) * 128], T.xs[:, sl, kc * 128:(kc + 1) * 128], T.ident_f,
                            [xr, R["c"][0]], [psr])
                dst = T.xT[:, h * 4:(h + 1) * 4, b * 128:(b + 1) * 128]
                src = pst[:, :].rearrange("p (q t) -> p q t", q=4)
                if h == 0:
                    self.P.add("act", lambda e, dst=dst, src=src: e.activation(out=dst, in_=src, func=AF.Copy),
                               [psr], [R["xT"][k] for k in range(h * 4, h * 4 + 4)])
                else:
                    self.cp(dst, src, [psr], [R["xT"][k] for k in range(h * 4, h * 4 + 4)])
                T.pspool.put((pst, psr))

    def store_tile(self, ti, pos, nb):
        T = self
        dr = T.dr
        R = T.R
        tot = NMETA + T.seq
        for b in range(nb):
            p0 = pos + b * 128
            lo = max(p0, NMETA)
            hi = min(p0 + 128, tot)
            if hi <= lo:
                continue
            sl = b % 2
            xr = R["xs"][sl]
            for h in range(2):
                pst, psr = T.pspool.get()
                for q in range(4):
                    kc = h * 4 + q
                    self.tr(pst[:, q * 128:(q + 1) * 128], T.xT[:, kc, b * 128:(b + 1) * 128], T.ident_f,
                            [R["xT"][kc], R["c"][0]], [psr])
                dst = T.xs[:, sl, h * 512:(h + 1) * 512]
                if h == 0:
                    self.P.add("act", lambda e, dst=dst, src=pst: e.activation(out=dst, in_=src[:, :], func=AF.Copy),
                               [psr], [xr])
                else:
                    self.cp(dst, pst[:, :], [psr], [xr])
                T.pspool.put((pst, psr))
            self.dma("sp", dr["out"][lo - NMETA:hi - NMETA, :], T.xs[lo - p0:hi - p0, sl, :], ("out", sl), [xr], [])

    def rmsnorm(self, l, gcol, nb):
        T = self
        R = T.R
        nt = nb * 128
        pst, psr = T.pspool.get()
        for kc in range(KC):
            bi, br = T.bpool.get()
            sq = T.scb[:, bi, 0:nt]
            self.act(sq, T.xT[:, kc, 0:nt], AF.Square, [R["xT"][kc]], [br])
            self.mm(pst[:, 0:nt], T.ones_b, sq, kc == 0, kc == KC - 1, [br, R["c"][0]], [psr])
            T.bpool.put((bi, br))
        fi, fr = T.fpool.get()
        rstd = T.scf[:, fi, 0:nt]
        self.rsqrt(rstd, pst[:, 0:nt], 1.0 / D, 1e-6, [psr], fr)
        T.pspool.put((pst, psr))
        for kc in range(KC):
            self.stt(T.hT[:, kc, 0:nt], T.xT[:, kc, 0:nt], T.pvt[:, l, gcol + kc:gcol + kc + 1], rstd, ALU.mult, ALU.mult,
                     [R["xT"][kc], fr, R["pv"][0]], [R["hT"][kc]])
        T.fpool.put((fi, fr))

    def ffn(self, l, which, nb):
        T = self
        dr = T.dr
        R = T.R
        nt = nb * 128
        pre = "ffn%d" % which
        w1 = dr[pre + "_w1"][l].rearrange("(k p) f -> p k f", p=128)
        w3 = dr[pre + "_w3"][l].rearrange("(k p) f -> p k f", p=128)
        w2 = dr[pre + "_w2"][l].rearrange("(c p) n -> p c n", p=128)
        self.rmsnorm(l, PV_F1G if which == 1 else PV_F2G, nb)
        hres = R["hT"]
        for half in range(2):
            for pi in range(6):
                nf = 2 if pi < 5 else 1
                f0 = (half * 11 + 2 * pi) * 128
                wdt = nf * 128

                def d1(base, wdt=wdt):
                    return base[:, 0:KC * wdt].rearrange("p (k f) -> p k f", k=KC)

                def d3(base, wdt=wdt):
                    return base[:, 2048:2048 + KC * wdt].rearrange("p (k f) -> p k f", k=KC)

                base, wres = self.piece([(d1, w1[:, :, f0:f0 + wdt]), (d3, w3[:, :, f0:f0 + wdt])])
                wa = d1(base)
                wb = d3(base)
                for j in range(nf):
                    fl = 2 * pi + j
                    pa, par = T.pspool.get()
                    pb, pbr = T.pspool.get()
                    for kc in range(KC):
                        self.mm(pa[:, 0:nt], wa[:, kc, j * 128:(j + 1) * 128], T.hT[:, kc, 0:nt], kc == 0, kc == KC - 1,
                                [wres, hres[kc]], [par])
                    for kc in range(KC):
                        self.mm(pb[:, 0:nt], wb[:, kc, j * 128:(j + 1) * 128], T.hT[:, kc, 0:nt], kc == 0, kc == KC - 1,
                                [wres, hres[kc]], [pbr])
                    fi, fr = T.fpool.get()
                    s = T.scf[:, fi, 0:nt]
                    self.sigmoid(s, pa[:, 0:nt], [par], fr)
                    self.tt(s, s, pa[:, 0:nt], ALU.mult, [fr, par], [fr])
                    self.tt(T.m[:, fl, 0:nt], s, pb[:, 0:nt], ALU.mult, [fr, pbr], [R["m"][fl]])
                    T.fpool.put((fi, fr))
                    T.pspool.put((pa, par))
                    T.pspool.put((pb, pbr))
            for pi in range(4):
                c0 = pi * 256

                def d2(base):
                    return base[:, 0:11 * 256].rearrange("p (c n) -> p c n", c=11)

                base, wres = self.piece([(d2, w2[:, half * 11:(half + 1) * 11, c0:c0 + 256])])
                ww = d2(base)
                for j in range(2):
                    dc = pi * 2 + j
                    py, pyr = T.pspool.get()
                    for fl in range(11):
                        self.mm(py[:, 0:nt], ww[:, fl, j * 128:(j + 1) * 128], T.m[:, fl, 0:nt], fl == 0, fl == 10,
                                [wres, R["m"][fl]], [pyr])
                    self.stt(T.xT[:, dc, 0:nt], py[:, 0:nt], 0.5, T.xT[:, dc, 0:nt], ALU.mult, ALU.add,
                             [pyr, R["xT"][dc]], [R["xT"][dc]])
                    T.pspool.put((py, pyr))

    def mixer(self, l, nb, first):
        T = self
        dr = T.dr
        R = T.R
        nt = nb * 128
        cres = R["c"][0]
        pvr = R["pv"][0]
        hres = R["hT"]
        win = dr["w_in"][l].rearrange("(k p) n -> p k n", p=128)
        self.rmsnorm(l, PV_MXG, nb)

        def std_piece(c0, wdt):
            def dd(base, wdt=wdt):
                return base[:, 0:KC * wdt].rearrange("p (k n) -> p k n", k=KC)
            base, wres = self.piece([(dd, win[:, :, c0:c0 + wdt])])
            return dd(base), wres

        def proj_fm(wv, wres, col0, out_ps, out_res):
            for kc in range(KC):
                self.mm(out_ps[:, 0:nt], wv[:, kc, col0:col0 + 128], T.hT[:, kc, 0:nt], kc == 0, kc == KC - 1,
                        [wres, hres[kc]], [out_res])

        def dq(base):
            return base[:, 0:4096].rearrange("p (k c g d) -> p k g c d", k=KC, c=4, g=2, d=64)
        srcq = win[:, :, 0:512].rearrange("p k (g c d) -> p k g c d", g=2, c=4, d=64)
        i, qwres = T.slots[T.slot_next % len(T.slots)]
        T.slot_next += 1
        qbase = T.ring[:, i, :]
        self.slot_dmas(i, qwres, [(dq(qbase)[:, :, g, c, :], srcq[:, :, g, c, :]) for g in range(2) for c in range(4)])
        wq = qbase[:, 0:4096].rearrange("p (k n) -> p k n", k=KC)
        wkv, kvres = std_piece(512, 256)

        def qknorm(src_ps, src_res, gcol, lnmul, dst, dst_res):
            bi, br = T.bpool.get()
            sq = T.scb[:, bi, 0:nt]
            self.act(sq, src_ps[:, 0:nt], AF.Square, [src_res], [br])
            p2, p2r = T.pspool.get()
            self.mm(p2[:, 0:nt], T.blk_b, sq, True, True, [br, cres], [p2r])
            T.bpool.put((bi, br))
            fi, fr = T.fpool.get()
            r = T.scf[:, fi, 0:nt]
            self.rsqrt(r, p2[:, 0:nt], 1.0, 64 * 1e-6, [p2r], fr, lnmul=lnmul)
            T.pspool.put((p2, p2r))
            self.stt(dst, src_ps[:, 0:nt], T.pvt[:, l, gcol:gcol + 1], r, ALU.mult, ALU.mult, [src_res, fr, pvr], [dst_res])
            T.fpool.put((fi, fr))

        for c in range(4):
            pq, pqr = T.pspool.get()
            proj_fm(wq, qwres, c * 128, pq, pqr)
            qknorm(pq, pqr, PV_GQ, 0.0, T.qn[:, c, 0:nt], R["qn"][c])
            T.pspool.put((pq, pqr))
        pk, pkr = T.pspool.get()
        proj_fm(wkv, kvres, 0, pk, pkr)
        qknorm(pk, pkr, PV_GK, math.log(8.0), T.kT[:, l, 128:128 + nt], R["kT"][l])
        T.pspool.put((pk, pkr))
        for b in range(nb):
            pv_, pvr_ = T.pspool.get()
            for kc in range(KC):
                self.mm(pv_[:, 0:128], T.hT[:, kc, b * 128:(b + 1) * 128], wkv[:, kc, 128:256], kc == 0, kc == KC - 1,
                        [kvres, hres[kc]], [pvr_])
            self.cp(T.vtk[:, l, 1 + b, :], pv_[:, 0:128], [pvr_], [R["vtk"][l]])
            T.pspool.put((pv_, pvr_))
        ktr = R["kT"][l]
        vtr = R["vtk"][l]
        for qb in range(nb):
            has_prev = not (first and qb == 0)
            for g in range(2):
                gs_ = slice(g * 64, (g + 1) * 64)
                rhs_q = T.qn[gs_, :, qb * 128:(qb + 1) * 128]
                blocks = [(1, T.mown4)]
                if has_prev:
                    blocks.append((0, T.mprev4))
                ptiles = []
                for (own, mask) in blocks:
                    kcol = (128 + qb * 128) if own else (qb * 128)
                    pss, pssr = T.pspool.get()
                    self.mm(pss[:, :], T.kT[gs_, l, kcol:kcol + 128], rhs_q, True, True, [ktr] + R["qn"], [pssr])
                    bi, br = T.bpool.get()
                    pt = T.scb[:, bi, 0:512]
                    self.act(pt, pss[:, :], AF.Exp, [pssr], [br])
                    T.pspool.put((pss, pssr))
                    self.tt(pt, pt, mask, ALU.mult, [br, cres], [br])
                    ptiles.append((own, pt, bi, br))
                po, por = T.pspool.get()
                pd, pdr = T.pspool.get()
                n = len(ptiles)
                for ii, (own, pt, bi, br) in enumerate(ptiles):
                    vb = (1 + qb) if own else qb
                    self.mm(po[:, :], T.vtk[:, l, vb, :], pt, ii == 0, ii == n - 1, [vtr, br], [por])
                for ii, (own, pt, bi, br) in enumerate(ptiles):
                    self.mm(pd[:, :], T.ones_b, pt, ii == 0, ii == n - 1, [cres, br], [pdr])
                for (own, pt, bi, br) in ptiles:
                    T.bpool.put((bi, br))
                fi, fr = T.fpool.get()
                dt_ = T.scf[gs_, fi, 0:512]
                self.tt(dt_, pd[gs_, :], T.sinkx[gs_, l, :], ALU.add, [pdr, pvr], [fr])
                self.recip(dt_, dt_, [fr], [fr])
                T.pspool.put((pd, pdr))
                self.tt(T.oa[gs_, :, qb * 128:(qb + 1) * 128], po[gs_, :].rearrange("p (j q) -> p j q", j=4),
                        dt_.rearrange("p (j q) -> p j q", j=4), ALU.mult, [por, fr], R["oa"])
                T.fpool.put((fi, fr))
                T.pspool.put((po, por))
        self.act(T.kT[:, l, 0:128], T.kT[:, l, nt:nt + 128], AF.Copy, [ktr], [ktr])
        self.act(T.vtk[:, l, 0, :], T.vtk[:, l, nb, :], AF.Copy, [vtr], [vtr])

        if STOP == "attn":
            self.stopped = True
            return
        for pi in range(2):
            def da(base):
                return base[:, 0:2048].rearrange("p (k n) -> p k n", k=KC)

            def dg(base):
                return base[:, 2048:4096].rearrange("p (k n) -> p k n", k=KC)
            base, wres = self.piece([(da, win[:, :, 768 + pi * 256:768 + pi * 256 + 256]),
                                     (dg, win[:, :, 1280 + pi * 256:1280 + pi * 256 + 256])])
            wa, wg = da(base), dg(base)
            for j in range(2):
                c = pi * 2 + j
                pa, par = T.pspool.get()
                pg, pgr = T.pspool.get()
                proj_fm(wa, wres, j * 128, pa, par)
                proj_fm(wg, wres, j * 128, pg, pgr)
                fi, fr = T.fpool.get()
                s = T.scf[:, fi, 0:nt]
                self.sigmoid(s, pg[:, 0:nt], [pgr], fr)
                T.pspool.put((pg, pgr))
                self.tt(T.u[:, c, 30:30 + nt], s, pa[:, 0:nt], ALU.mult, [fr, par], [R["u"][c]])
                self.act(T.u[:, c, 0:30], T.uh[:, l, c, :], AF.Copy, [R["uh"][l]], [R["u"][c]])
                T.fpool.put((fi, fr))
                T.pspool.put((pa, par))
        def conv_tap(j, c):
            ur = R["u"][c]
            ar = R["acc"][c]
            wc = PV_CW + c * 31 + j
            eng = "pool" if c in POOL_CHUNKS else "dve"
            if j == 0:
                self.ts(T.acc[:, c, 0:nt], T.u[:, c, 0:nt], T.pvt[:, l, wc:wc + 1], T.pvt[:, l, PV_CB + c:PV_CB + c + 1],
                        ALU.mult, ALU.add, [ur, pvr], [ar], eng=eng)
            else:
                self.stt(T.acc[:, c, 0:nt], T.u[:, c, j:j + nt], T.pvt[:, l, wc:wc + 1], T.acc[:, c, 0:nt],
                         ALU.mult, ALU.add, [ur, pvr, ar], [ar], eng=eng)
        for j in range(31):
            for c in range(4):
                self.defer(lambda j=j, c=c: conv_tap(j, c))

        def conv_hist(c):
            ur = R["u"][c]
            self.act(T.uh[:, l, c, :], T.u[:, c, nt:nt + 30], AF.Copy, [ur], [R["uh"][l]])
        for c in range(4):
            self.defer(lambda c=c: conv_hist(c))

        def ln_all():
            pm, pmr = T.pspool.get()
            for c in range(4):
                bi, br = T.bpool.get()
                ab = T.scb[:, bi, 0:nt]
                self.act(ab, T.acc[:, c, 0:nt], AF.Copy, [R["acc"][c]], [br])
                self.mm(pm[:, 0:nt], T.ones_b, ab, c == 0, c == 3, [br, cres], [pmr])
                T.bpool.put((bi, br))
            for c in range(4):
                self.stt(T.acc[:, c, 0:nt], pm[:, 0:nt], -1.0 / 512, T.acc[:, c, 0:nt], ALU.mult, ALU.add,
                         [pmr, R["acc"][c]], [R["acc"][c]])
            T.pspool.put((pm, pmr))
            pv2, pv2r = T.pspool.get()
            for c in range(4):
                bi, br = T.bpool.get()
                sq = T.scb[:, bi, 0:nt]
                self.act(sq, T.acc[:, c, 0:nt], AF.Square, [R["acc"][c]], [br])
                self.mm(pv2[:, 0:nt], T.ones_b, sq, c == 0, c == 3, [br, cres], [pv2r])
                T.bpool.put((bi, br))
            fi, fr = T.fpool.get()
            rstd = T.scf[:, fi, 0:nt]
            self.rsqrt(rstd, pv2[:, 0:nt], 1.0 / 512, 1e-5, [pv2r], fr)
            T.pspool.put((pv2, pv2r))
            for c in range(4):
                ar = R["acc"][c]
                self.tt(T.acc[:, c, 0:nt], T.acc[:, c, 0:nt], rstd, ALU.mult, [ar, fr], [ar])
                self.act(T.acc[:, c, 0:nt], T.acc[:, c, 0:nt], AF.Identity, [ar, pvr], [ar],
                         scale=T.pvt[:, l, PV_LG + c:PV_LG + c + 1], bias=T.pvt[:, l, PV_LB + c:PV_LB + c + 1])
                f2, f2r = T.fpool.get()
                s_ = T.scf[:, f2, 0:nt]
                self.sigmoid(s_, T.acc[:, c, 0:nt], [ar], f2r)
                self.tt(T.cb[:, c, 0:nt], s_, T.acc[:, c, 0:nt], ALU.mult, [f2r, ar], [R["cb"][c]])
                T.fpool.put((f2, f2r))
            T.fpool.put((fi, fr))
        self.defer(ln_all)
        if STOP == "conv":
            self.pump()

        if STOP == "conv":
            self.stopped = True
            return
        hist = R["qkh"][l]
        for qk in range(2):
            wv, wres = std_piece(1792 + qk * 512, 512)
            for c in range(4):
                ch = qk * 4 + c
                pq, pqr = T.pspool.get()
                proj_fm(wv, wres, c * 128, pq, pqr)
                rs = ch % 2
                rr = R["raw"][rs]
                self.P.add("act", lambda e, o=T.raw[:, rs, 3:3 + nt], i_=pq[:, 0:nt]: e.activation(out=o, in_=i_, func=AF.Copy),
                           [pqr], [rr])
                T.pspool.put((pq, pqr))
                self.act(T.raw[:, rs, 0:3], T.qkh[:, l, ch, :], AF.Copy, [hist], [rr])
                car = R["cacc"][rs]
                wc = PV_QW + ch * 4
                self.ts(T.cacc[:, rs, 0:nt], T.raw[:, rs, 0:nt], T.pvt[:, l, wc:wc + 1], T.pvt[:, l, PV_QB + ch:PV_QB + ch + 1],
                        ALU.mult, ALU.add, [rr, pvr], [car])
                for j in range(1, 4):
                    self.stt(T.cacc[:, rs, 0:nt], T.raw[:, rs, j:j + nt], T.pvt[:, l, wc + j:wc + j + 1], T.cacc[:, rs, 0:nt],
                             ALU.mult, ALU.add, [rr, pvr, car], [car])
                self.act(T.qkh[:, l, ch, :], T.raw[:, rs, nt:nt + 3], AF.Copy, [rr], [hist])
                fi, fr = T.fpool.get()
                s = T.scf[:, fi, 0:nt]
                self.sigmoid(s, T.cacc[:, rs, 0:nt], [car], fr)
                self.tt(T.mqk[:, ch, 0:nt], s, T.cacc[:, rs, 0:nt], ALU.mult, [fr, car], [R["mqk"][ch]])
                T.fpool.put((fi, fr))
                self.pump(7)
        for b in range(nb):
            pst, psr = T.pspool.get()
            pstb = pst.bitcast(BF16)
            for h in range(4):
                self.tr(pstb[:, h * 128:(h + 1) * 128], T.mqk[:, 4 + h, b * 128:(b + 1) * 128], T.ident_b,
                        [R["mqk"][4 + h], cres], [psr])
            self.cp(T.ktok[:, b, :, :], pstb[:, 0:512].rearrange("p (h d) -> p h d", h=4), [psr], [R["ktok"][b]])
            T.pspool.put((pst, psr))
            self.pump(2)
        wv, wres = std_piece(2816, 512)
        for b in range(nb):
            pv_, pvr_ = T.pspool.get()
            for kc in range(KC):
                self.mm(pv_[:, :], T.hT[:, kc, b * 128:(b + 1) * 128], wv[:, kc, 0:512], kc == 0, kc == KC - 1,
                        [wres, hres[kc]], [pvr_])
            self.P.add("act", lambda e, o=T.vaug[:, b, :, 0:128], i_=pv_[:, :].rearrange("p (h d) -> p h d", h=4):
                       e.activation(out=o, in_=i_, func=AF.Copy), [pvr_], [R["vaug"][b]])
            T.pspool.put((pv_, pvr_))
        wv, wres = std_piece(3328, 512)
        for c in range(4):
            po, por = T.pspool.get()
            proj_fm(wv, wres, c * 128, po, por)
            fi, fr = T.fpool.get()
            s = T.scf[:, fi, 0:nt]
            self.sigmoid(s, po[:, 0:nt], [por], fr)
            T.pspool.put((po, por))
            self.cp(T.so[:, c, 0:nt], s, [fr], [R["so"][c]])
            T.fpool.put((fi, fr))
            self.pump(4)
        if STOP == "m3":
            self.stopped = True
            return
        def dgt(base):
            return base[:, 0:1024].rearrange("p (k n) -> p k n", k=KC)
        gbase, gwres = self.piece([(dgt, win[:, :, 3840:3968])])
        wgt = dgt(gbase)
        Cr, Cbr, nbr = R["Cst"][l], R["Cbf"][l], R["nbc"][l]
        isq = 1.0 / math.sqrt(128.0)
        Pm_all = T.m[:, 0:4, :]
        qs_all = T.m[:, 4:8, :]
        for b in range(nb):
            cs = slice(b * 128, (b + 1) * 128)
            smr = R["sm"][b]
            sm = T.sm[:, b * 4:(b + 1) * 4, :]
            pg, pgr = T.pspool.get()
            for kc in range(KC):
                self.mm(pg[:, 0:16], T.hT[:, kc, cs], wgt[:, kc, 0:16], kc == 0, kc == KC - 1, [gwres, hres[kc]], [pgr])
            self.tt(sm[:, 0, :], pg[:, 0:8], T.pvt[:, l, PV_GT:PV_GT + 8], ALU.add, [pgr, pvr], [smr])
            T.pspool.put((pg, pgr))
            self.act(sm[:, 1, 0:4], sm[:, 0, 4:8], AF.Exp, [smr], [smr], scale=-1.0)
            self.act(sm[:, 1, 0:4], sm[:, 1, 0:4], AF.Ln, [smr], [smr], bias=1.0)
            fi, fr = T.fpool.get()
            nlb = T.scf[:, fi, 0:512]
            for h in range(4):
                self.ts(nlb[:, h * 128:(h + 1) * 128], T.ones_f, sm[:, 1, h:h + 1], None, ALU.mult, None, [cres, smr], [fr])
            pnb, pnbr = T.pspool.get()
            for h in range(4):
                self.mm(pnb[:, h * 128:(h + 1) * 128], nlb[:, h * 128:(h + 1) * 128], T.tri_f, True, True, [fr, cres], [pnbr])
            self.tt(nlb, pnb[:, :], T.ident4_f, ALU.mult, [pnbr, cres], [fr])
            self.P.add("dve", lambda e, o=sm[:, 1, 4:8], i_=nlb.rearrange("p (h t) -> p h t", h=4):
                       e.reduce_sum(out=o, in_=i_, axis=mybir.AxisListType.X), [fr], [smr])
            self.tt(sm[:, 1, 4:8], sm[:, 1, 4:8], sm[:, 0, 0:4], ALU.add, [smr], [smr])
            T.fpool.put((fi, fr))
            fd, fdr = T.fpool.get()
            Dm = T.scf[:, fd, 0:512]
            self.tt(Dm, T.maskb4, pnb[:, :], ALU.subtract, [cres, pnbr], [fdr])
            for h in range(4):
                self.act(Dm[:, h * 128:(h + 1) * 128], Dm[:, h * 128:(h + 1) * 128], AF.Exp, [fdr, smr], [fdr],
                         bias=sm[:, 1, 4 + h:5 + h])
            fe, fer = T.fpool.get()
            eb = T.scf[:, fe, 0:512]
            self.act(eb, pnb[:, :], AF.Exp, [pnbr], [fer], scale=-1.0)
            T.pspool.put((pnb, pnbr))
            self.act(sm[:, 2, 0:4], sm[:, 1, 4:8], AF.Exp, [smr], [smr])
            ebv = eb.rearrange("p (h t) -> p h t", h=4)
            self.cp(sm[:, 2, 4:8], ebv[:, :, 127], [fer], [smr])
            self.tt(sm[:, 3, 0:4], sm[:, 2, 0:4], sm[:, 2, 4:8], ALU.mult, [smr], [smr])
            self.stt(qs_all[:, b, :].rearrange("p (h t) -> p h t", h=4), T.mqk[:, 0:4, cs], isq, ebv,
                     ALU.mult, ALU.mult, R["mqk"][0:4] + [fer], [R["m"][4 + b]])
            T.fpool.put((fe, fer))
            pgm, pgmr = T.pspool.get()
            for h in range(4):
                self.mm(pgm[:, h * 128:(h + 1) * 128], T.mqk[:, 4 + h, cs], T.mqk[:, h, cs], True, True,
                        [R["mqk"][4 + h], R["mqk"][h]], [pgmr])
            self.stt(Pm_all[:, b, :], pgm[:, :], isq, Dm, ALU.mult, ALU.mult, [pgmr, fdr], [R["m"][b]])
            T.pspool.put((pgm, pgmr))
            T.fpool.put((fd, fdr))
            vw = T.vw[:, b, :].rearrange("p (h e) -> p h e", h=4)
            for h in range(4):
                self.ts(vw[:, h, 0:129], T.vaug[:, b, h, 0:129], sm[:, 3, h:h + 1], None, ALU.mult, None,
                        [R["vaug"][b], smr], [R["vw"][b]])
            self.pump(10)
        self.pump()
        for b in range(nb):
            cs = slice(b * 128, (b + 1) * 128)
            smr = R["sm"][b]
            sm = T.sm[:, b * 4:(b + 1) * 4, :]
            Pm = Pm_all[:, b, :]
            qs = qs_all[:, b, :]
            bpr, bqr = R["m"][b], R["m"][4 + b]
            vw = T.vw[:, b, :].rearrange("p (h e) -> p h e", h=4)
            pn, pnr = T.pspool.get()
            pd, pdr = T.pspool.get()
            for h in range(4):
                hs = slice(h * 128, (h + 1) * 128)
                self.mm(pn[:, hs], T.vaug[:, b, h, 0:128], Pm[:, hs], True, False, [R["vaug"][b], bpr], [pnr])
                self.mm(pn[:, hs], T.Cbf[:, l, h, :], qs[:, hs], False, True, [Cbr, bqr], [pnr])
            for h in range(4):
                hs = slice(h * 128, (h + 1) * 128)
                self.mm(pd[:, hs], T.ones_b, Pm[:, hs], True, False, [cres, bpr], [pdr])
                self.mm(pd[:, hs], T.nbc[:, l, h, :], qs[:, hs], False, True, [nbr, bqr], [pdr])
            pc0, pc0r = T.pspool.get()
            pc1, pc1r = T.pspool.get()
            for h in range(4):
                pcc, pccr = (pc0, pc0r) if h < 2 else (pc1, pc1r)
                o = (h % 2) * 130
                self.mm(pcc[:, o:o + 129], T.ktok[:, b, h, :], vw[:, h, 0:129], True, True, [R["ktok"][b], R["vw"][b]], [pccr])
            for h in range(4):
                pcc, pccr = (pc0, pc0r) if h < 2 else (pc1, pc1r)
                o = (h % 2) * 130
                self.stt(T.Cst[:, l, h, :], T.Cst[:, l, h, :], sm[:, 2, 4 + h:5 + h], pcc[:, o:o + 129],
                         ALU.mult, ALU.add, [Cr, smr, pccr], [Cr])
            T.pspool.put((pc0, pc0r))
            T.pspool.put((pc1, pc1r))
            self.P.add("act", lambda e, o=T.Cbf[:, l, :, :], i_=T.Cst[:, l, :, 0:128]: e.activation(out=o, in_=i_, func=AF.Copy),
                       [Cr], [Cbr])
            for h in range(4):
                self.act(T.nbc[:, l, h, :], T.ones_f, AF.Identity, [cres, Cr], [nbr], scale=T.Cst[:, l, h, 128:129])
            fn_, fnr = T.fpool.get()
            dn = T.scf[:, fn_, 0:512]
            self.act(dn, pd[:, :], AF.Abs, [pdr], [fnr])
            T.pspool.put((pd, pdr))
            self.ts(dn, dn, 1.0, None, ALU.max, None, [fnr], [fnr])
            self.recip(dn, dn, [fnr], [fnr])
            self.tt(dn, pn[:, :], dn, ALU.mult, [pnr, fnr], [fnr])
            T.pspool.put((pn, pnr))
            self.tt(T.hm[:, :, cs], dn.rearrange("p (h t) -> p h t", h=4), T.so[:, :, cs], ALU.mult, [fnr] + R["so"], R["hm"])
            T.fpool.put((fn_, fnr))

        self.pump()
        if STOP in ("mlstm", "m4", "g1", "g2", "g3", "g4a", "g4b"):
            self.stopped = True
            return
        woa = dr["w_o_attn"][l]
        woc = dr["w_o_conv"][l].rearrange("(j p) n -> p j n", p=128)
        wom = dr["w_o_mlstm"][l].rearrange("(j p) n -> p j n", p=128)
        zg = win[:, :, 3848:6920].rearrange("p k (b n) -> p b k n", b=3)
        for dc in range(8):
            ds_ = slice(dc * 128, (dc + 1) * 128)

            def dz(base):
                return base[:, 0:3072].rearrange("p (b k n) -> p b k n", b=3, k=KC)
            iz, zres = T.slots[T.slot_next % len(T.slots)]
            T.slot_next += 1
            zbase = T.ring[:, iz, :]
            wz = dz(zbase)
            self.slot_dmas(iz, zres, [(wz[:, b_, :, :], zg[:, b_, :, ds_]) for b_ in range(3)])
            i2, ores = T.slots[T.slot_next % len(T.slots)]
            T.slot_next += 1
            obase = T.ring[:, i2, :]
            wo = obase[:, 0:1536].rearrange("p (b j n) -> p b j n", b=3, j=4)
            prs = []
            for g in range(2):
                src = woa[g * 256:(g + 1) * 256, ds_].rearrange("(j d) n -> d j n", d=64)
                prs.append((wo[g * 64:(g + 1) * 64, 0, :, :], src))
            prs.append((wo[:, 1, :, :], woc[:, :, ds_]))
            prs.append((wo[:, 2, :, :], wom[:, :, ds_]))
            self.slot_dmas(i2, ores, prs)
            gts = []
            for br_ in range(3):
                pz, pzr = T.pspool.get()
                for kc in range(KC):
                    self.mm(pz[:, 0:nt], wz[:, br_, kc, :], T.hT[:, kc, 0:nt], kc == 0, kc == KC - 1, [zres, hres[kc]], [pzr])
                fi, fr = T.fpool.get()
                s = T.scf[:, fi, 0:nt]
                self.sigmoid(s, pz[:, 0:nt], [pzr, pvr], fr, negbias=T.ngb[:, l, br_ * 8 + dc:br_ * 8 + dc + 1])
                T.pspool.put((pz, pzr))
                gts.append((s, fi, fr))
            srcs = [(T.oa, R["oa"]), (T.cb, R["cb"]), (T.hm, R["hm"])]
            for br_ in range(3):
                py, pyr = T.pspool.get()
                buf, bres = srcs[br_]
                for j in range(4):
                    self.mm(py[:, 0:nt], wo[:, br_, j, :], buf[:, j, 0:nt], j == 0, j == 3, [ores, bres[j]], [pyr])
                s, fi, fr = gts[br_]
                self.tt(s, s, py[:, 0:nt], ALU.mult, [fr, pyr], [fr])
                T.pspool.put((py, pyr))
            s0, f0, r0 = gts[0]
            s1, f1, r1 = gts[1]
            s2, f2, r2 = gts[2]
            self.tt(s0, s0, s1, ALU.add, [r0, r1], [r0])
            self.tt(T.mg[:, dc, 0:nt], s0, s2, ALU.add, [r0, r2], [R["mg"][dc]])
            for (s, fi, fr) in gts:
                T.fpool.put((fi, fr))
        wout = dr["w_out"][l].rearrange("(k p) n -> p k n", p=128)
        for pi in range(2):
            def dd(base):
                return base[:, 0:4096].rearrange("p (k n) -> p k n", k=KC)
            base, wres = self.piece([(dd, wout[:, :, pi * 512:(pi + 1) * 512])])
            wv = dd(base)
            for j in range(4):
                dc = pi * 4 + j
                py, pyr = T.pspool.get()
                for kc in range(KC):
                    self.mm(py[:, 0:nt], wv[:, kc, j * 128:(j + 1) * 128], T.mg[:, kc, 0:nt], kc == 0, kc == KC - 1,
                            [wres, R["mg"][kc]], [pyr])
                self.tt(T.xT[:, dc, 0:nt], py[:, 0:nt], T.xT[:, dc, 0:nt], ALU.add, [pyr, R["xT"][dc]], [R["xT"][dc]])
                T.pspool.put((py, pyr))


def make_consts():
    c32 = np.zeros((128, 1408), np.float32)
    r = np.arange(128)
    c32[:, 0:128] = np.eye(128, dtype=np.float32)
    tri = (r[:, None] <= r[None, :]).astype(np.float32)
    c32[:, 128:256] = tri
    maskb = np.where(r[:, None] <= r[None, :], 0.0, -30000.0).astype(np.float32)
    c32[:, 256:768] = np.tile(maskb, (1, 4))
    c32[:, 768:896] = 1.0
    c32[:, 896:1408] = np.tile(np.eye(128, dtype=np.float32), (1, 4))
    cbf = np.zeros((128, 1408), np.float32)
    cbf[:, 0:128] = 1.0
    blk = np.zeros((128, 128), np.float32)
    blk[0:64, 0:64] = 1.0
    blk[64:128, 64:128] = 1.0
    cbf[:, 128:256] = blk
    cbf[:, 256:384] = np.eye(128, dtype=np.float32)
    cbf[:, 384:896] = np.tile(tri, (1, 4))
    cbf[:, 896:1408] = np.tile(1.0 - tri, (1, 4))
    return c32, cbf


def make_pv(inp):
    L = DEPTH
    pv = np.zeros((L, 128, NV), np.float32)
    for l in range(L):
        def fm(v, n):
            return np.ascontiguousarray(np.asarray(v, np.float32).reshape(n, 128).T)
        pv[l, :, PV_F1G:PV_F1G + 8] = fm(inp["ffn1_norm"][l], 8)
        pv[l, :, PV_MXG:PV_MXG + 8] = fm(inp["mix_norm"][l], 8)
        pv[l, :, PV_F2G:PV_F2G + 8] = fm(inp["ffn2_norm"][l], 8)
        gb = np.asarray(inp["gate_bias"][l], np.float32)
        for b in range(3):
            pv[l, :, PV_GB + b * 8:PV_GB + b * 8 + 8] = fm(gb[b], 8)
        cw = np.asarray(inp["conv_dw_w"][l], np.float32)
        for c in range(4):
            pv[l, :, PV_CW + c * 31:PV_CW + (c + 1) * 31] = cw[:, c * 128:(c + 1) * 128].T
        pv[l, :, PV_CB:PV_CB + 4] = fm(inp["conv_dw_b"][l], 4)
        pv[l, :, PV_LG:PV_LG + 4] = fm(inp["conv_ln_g"][l], 4)
        pv[l, :, PV_LB:PV_LB + 4] = fm(inp["conv_ln_b"][l], 4)
        qw = np.asarray(inp["mlstm_qk_conv_w"][l], np.float32)
        for c in range(8):
            pv[l, :, PV_QW + c * 4:PV_QW + (c + 1) * 4] = qw[:, c * 128:(c + 1) * 128].T
        pv[l, :, PV_QB:PV_QB + 8] = fm(inp["mlstm_qk_conv_b"][l], 8)
        pv[l, :, PV_GQ] = np.tile(np.asarray(inp["attn_q_norm"][l], np.float32), 2)
        pv[l, :, PV_GK] = np.tile(np.asarray(inp["attn_k_norm"][l], np.float32), 2)
        sk = np.asarray(inp["attn_sinks"][l], np.float32)
        pv[l, 0:64, PV_SK:PV_SK + 4] = sk[None, 0:4]
        pv[l, 64:128, PV_SK:PV_SK + 4] = sk[None, 4:8]
        pv[l, :, PV_GT:PV_GT + 4] = np.asarray(inp["mlstm_igate_bias"][l], np.float32)[None, :]
        pv[l, :, PV_GT + 4:PV_GT + 8] = np.asarray(inp["mlstm_fgate_bias"][l], np.float32)[None, :]
    return pv


_NC_CACHE = {}


def run(inputs, n_cores=None, depth=DEPTH, trace=False):
    x = np.asarray(inputs["x"], np.float32)
    bsz, seq, _ = x.shape
    if n_cores is None:
        n_cores = bsz
    key = (seq, depth, STOP)
    if key not in _NC_CACHE:
        _NC_CACHE[key] = Builder(seq, depth).build()
    nc = _NC_CACHE[key]
    c32, cbf = make_consts()
    pv = make_pv(inputs)
    shared = {"meta": np.ascontiguousarray(np.asarray(inputs["meta_tokens"], np.float32)), "pv": pv, "c32": c32, "cbf": cbf}
    for k in ("ffn1_w1", "ffn1_w3", "ffn1_w2", "ffn2_w1", "ffn2_w3", "ffn2_w2", "w_in", "w_o_attn", "w_o_conv",
              "w_o_mlstm", "w_out"):
        shared[k] = np.ascontiguousarray(np.asarray(inputs[k], np.float32))
    in_maps = []
    for b in range(n_cores):
        d = dict(shared)
        d["xin"] = np.ascontiguousarray(x[b])
        in_maps.append(d)
    res = run_bass_kernel_spmd(nc, in_maps, core_ids=list(range(n_cores)), **({"trace": True} if trace else {}))
    out = np.stack([np.asarray(r["out"], np.float32) for r in res.results], axis=0)
    return out, res


def kernel(**inputs):
    out, _ = run(inputs)
    return out
```
